# Optimizing a Trainium2 kernel written in Bass

```python
import jax, jax.numpy as jnp
from jax import lax
import numpy as np

D_MODEL = 2048
BATCH = 8
SEQ = 2048
DEPTH = 2

N_MIXERS = 2
HEAD_DIM = 128
HEADS_PER_GROUP = 4
DILATED_PATTERNS = ((128, 1), (512, 4), (2048, 16))
N_ATTN_GROUPS = len(DILATED_PATTERNS)
N_HEADS = HEADS_PER_GROUP * N_ATTN_GROUPS
ATTN_WIDTH = N_HEADS * HEAD_DIM
ROT_DIM = HEAD_DIM // 4
ROPE_THETA = 500000.0
ATTN_BLOCK = 64
NEG_INF = -1e30
FOURIER_GROUPS = 8
FOURIER_GROUP_DIM = D_MODEL // FOURIER_GROUPS
N_EXPERT_GROUPS = 8
EXPERTS_PER_GROUP = 8
N_EXPERTS = N_EXPERT_GROUPS * EXPERTS_PER_GROUP
TOP_K_INNER = 2
D_EXPERT = D_MODEL // 4
MOE_BLOCK = 128
EPS = 1e-6
N_ATTN_LAYERS = (DEPTH + 1) // 2
N_FOURIER_LAYERS = DEPTH // 2

kernel_name = "hybrid_dilated_fourier_hmoe_encoder"


def rmsnorm(x, g):
    xf = x.astype(jnp.float32)
    y = xf * lax.rsqrt(jnp.mean(xf * xf, axis=-1, keepdims=True) + EPS)
    return (y * g.astype(jnp.float32)).astype(x.dtype)


def partial_rope(t, seq_len):
    half = ROT_DIM // 2
    inv_freq = ROPE_THETA ** (-jnp.arange(0, ROT_DIM, 2, dtype=jnp.float32) / ROT_DIM)
    ang = jnp.arange(seq_len, dtype=jnp.float32)[:, None] * inv_freq[None, :]
    cos = jnp.cos(ang)[None, :, None, :]
    sin = jnp.sin(ang)[None, :, None, :]
    tf = t.astype(jnp.float32)
    t1, t2, rest = tf[..., :half], tf[..., half:ROT_DIM], tf[..., ROT_DIM:]
    out = jnp.concatenate([t1 * cos - t2 * sin, t2 * cos + t1 * sin, rest], axis=-1)
    return out.astype(t.dtype)


def dilated_window_attention(q, k, v, window, dilation):
    b_, s_, h_, dh = q.shape
    radius = (window // 2) // dilation
    length = s_ // dilation
    nb = -(-length // ATTN_BLOCK)
    lp = nb * ATTN_BLOCK
    kspan = ATTN_BLOCK + 2 * radius

    def phase(t):
        return t.reshape(b_, length, dilation, h_, dh).transpose(0, 2, 3, 1, 4).astype(jnp.float32)

    qs, ks, vs = phase(q), phase(k), phase(v)
    qb = jnp.pad(qs, ((0, 0), (0, 0), (0, 0), (0, lp - length), (0, 0)))
    qb = qb.reshape(b_, dilation, h_, nb, ATTN_BLOCK, dh)
    pad_kv = ((0, 0), (0, 0), (0, 0), (radius, lp - length + radius), (0, 0))
    idx = np.arange(nb)[:, None] * ATTN_BLOCK + np.arange(kspan)[None, :]
    kb = jnp.take(jnp.pad(ks, pad_kv), idx, axis=3)
    vb = jnp.take(jnp.pad(vs, pad_kv), idx, axis=3)

    jq = np.arange(nb)[:, None] * ATTN_BLOCK + np.arange(ATTN_BLOCK)[None, :]
    jk = np.arange(nb)[:, None] * ATTN_BLOCK + np.arange(kspan)[None, :] - radius
    valid = (np.abs(jk[:, None, :] - jq[:, :, None]) <= radius) & (jk[:, None, :] >= 0) & (jk[:, None, :] < length)

    s = jnp.einsum('bdhnqc,bdhnkc->bdhnqk', qb, kb) * (dh ** -0.5)
    s = jnp.where(jnp.asarray(valid), s, NEG_INF)
    m = jnp.max(s, axis=-1, keepdims=True)
    p = jnp.exp(s - m)
    l = jnp.sum(p, axis=-1, keepdims=True)
    o = jnp.einsum('bdhnqk,bdhnkc->bdhnqc', p, vb) / l
    lse = (m + jnp.log(l))[..., 0]
    o = o.reshape(b_, dilation, h_, lp, dh)[:, :, :, :length]
    o = o.transpose(0, 3, 1, 2, 4).reshape(b_, s_, h_, dh)
    lse = lse.reshape(b_, dilation, h_, lp)[..., :length].transpose(0, 3, 1, 2).reshape(b_, s_, h_)
    return o, lse


def dilated_attention_mixer(x, norm_g, w_qkv, q_g, k_g, w_out):
    b_, s_, _ = x.shape
    h = rmsnorm(x, norm_g)
    qkv = jnp.einsum('bsd,de->bse', h, w_qkv)
    q, k, v = jnp.split(qkv, 3, axis=-1)
    q = q.reshape(b_, s_, N_HEADS, HEAD_DIM)
    k = k.reshape(b_, s_, N_HEADS, HEAD_DIM)
    v = v.reshape(b_, s_, N_HEADS, HEAD_DIM)
    q = partial_rope(rmsnorm(q, q_g), s_)
    k = partial_rope(rmsnorm(k, k_g), s_)
    outs, lses = [], []
    for gi, (window, dilation) in enumerate(DILATED_PATTERNS):
        sl = slice(gi * HEADS_PER_GROUP, (gi + 1) * HEADS_PER_GROUP)
        o, lse = dilated_window_attention(q[:, :, sl], k[:, :, sl], v[:, :, sl], window, dilation)
        outs.append(o)
        lses.append(lse)
    outs = jnp.stack(outs, axis=2)
    alpha = jax.nn.softmax(jnp.stack(lses, axis=2), axis=2)
    mixed = (outs * alpha[..., None]).reshape(b_, s_, ATTN_WIDTH).astype(x.dtype)
    return jnp.einsum('bse,ed->bsd', mixed, w_out)


def fourier_mixer(x, norm_g, w_in, w_out):
    b_, s_, _ = x.shape
    h = rmsnorm(x, norm_g)
    u = jnp.einsum('bsd,de->bse', h, w_in).astype(jnp.float32)
    u = u.reshape(b_, s_, FOURIER_GROUPS, FOURIER_GROUP_DIM)
    f = jnp.fft.fft2(u, axes=(1, 3), norm='ortho').real
    f = f.reshape(b_, s_, D_MODEL).astype(x.dtype)
    return jnp.einsum('bse,ed->bsd', f, w_out)


def hierarchical_moe(x, norm_g, w_rg, b_rg, w_re, b_re, w_gate, w_up, w_down):
    b_, s_, d_ = x.shape
    n = b_ * s_
    h = rmsnorm(x, norm_g).reshape(n, d_)
    hf = h.astype(jnp.float32)
    coarse = hf @ w_rg.astype(jnp.float32) + b_rg.astype(jnp.float32)
    g_sel = jnp.argmax(coarse, axis=-1)
    g_gate = jnp.take_along_axis(jax.nn.softmax(coarse, axis=-1), g_sel[:, None], axis=-1)
    fine = (hf @ w_re.astype(jnp.float32) + b_re.astype(jnp.float32)).reshape(n, N_EXPERT_GROUPS, EXPERTS_PER_GROUP)
    fine = jnp.take_along_axis(fine, g_sel[:, None, None], axis=1)[:, 0]
    top_v, top_i = lax.top_k(fine, TOP_K_INNER)
    weights = g_gate * jax.nn.softmax(top_v, axis=-1)
    experts = g_sel[:, None] * EXPERTS_PER_GROUP + top_i

    n_assign = n * TOP_K_INNER
    e_flat = experts.reshape(-1).astype(jnp.int32)
    w_flat = weights.reshape(-1)
    tok_flat = jnp.repeat(jnp.arange(n, dtype=jnp.int32), TOP_K_INNER)
    counts = jnp.zeros((N_EXPERTS,), jnp.int32).at[e_flat].add(1)
    padded = ((counts + MOE_BLOCK - 1) // MOE_BLOCK) * MOE_BLOCK
    pend = jnp.cumsum(padded)
    pstart = pend - padded
    start = jnp.cumsum(counts) - counts
    order = jnp.argsort(e_flat, stable=True)
    se = e_flat[order]
    dest = pstart[se] + (jnp.arange(n_assign, dtype=jnp.int32) - start[se])
    n_blocks = -(-n_assign // MOE_BLOCK) + N_EXPERTS
    cap = n_blocks * MOE_BLOCK
    slot_tok = jnp.full((cap,), n, jnp.int32).at[dest].set(tok_flat[order])
    slot_w = jnp.zeros((cap,), jnp.float32).at[dest].set(w_flat[order])
    block_start = jnp.arange(n_blocks, dtype=jnp.int32) * MOE_BLOCK
    block_e = jnp.minimum(jnp.searchsorted(pend, block_start, side='right'), N_EXPERTS - 1)

    h_pad = jnp.concatenate([h, jnp.zeros((1, d_), h.dtype)], axis=0)
    xs = h_pad[slot_tok].reshape(n_blocks, MOE_BLOCK, d_)

    def expert_block(args):
        xb, e = args
        a = jax.nn.silu(xb @ w_gate[e]) * (xb @ w_up[e])
        return a @ w_down[e]

    ys = lax.map(expert_block, (xs, block_e)).reshape(cap, d_)
    ys = ys * slot_w[:, None].astype(ys.dtype)
    out = jnp.zeros((n + 1, d_), ys.dtype).at[slot_tok].add(ys)[:n]
    return out.reshape(b_, s_, d_).astype(x.dtype)


def setup_inputs(seed: int = 0) -> dict:
    key = jax.random.key(seed)
    ks = jax.random.split(key, 20)
    f32 = jnp.float32
    nrm = lambda k, shape, scale: jax.random.normal(k, shape, f32) * scale
    gain = lambda k, shape: 1.0 + 0.02 * jax.random.normal(k, shape, f32)
    return {
        "x": jax.random.normal(ks[0], (BATCH, SEQ, D_MODEL), f32),
        "attn_norm_g": gain(ks[1], (N_ATTN_LAYERS, D_MODEL)),
        "w_qkv": nrm(ks[2], (N_ATTN_LAYERS, D_MODEL, 3 * ATTN_WIDTH), D_MODEL ** -0.5),
        "q_norm_g": gain(ks[3], (N_ATTN_LAYERS, HEAD_DIM)),
        "k_norm_g": gain(ks[4], (N_ATTN_LAYERS, HEAD_DIM)),
        "w_attn_out": nrm(ks[5], (N_ATTN_LAYERS, ATTN_WIDTH, D_MODEL), ATTN_WIDTH ** -0.5),
        "fourier_norm_g": gain(ks[6], (N_FOURIER_LAYERS, D_MODEL)),
        "w_fourier_in": nrm(ks[7], (N_FOURIER_LAYERS, D_MODEL, D_MODEL), D_MODEL ** -0.5),
        "w_fourier_out": nrm(ks[8], (N_FOURIER_LAYERS, D_MODEL, D_MODEL), D_MODEL ** -0.5),
        "moe_norm_g": gain(ks[9], (DEPTH, D_MODEL)),
        "w_router_group": nrm(ks[10], (DEPTH, D_MODEL, N_EXPERT_GROUPS), D_MODEL ** -0.5),
        "b_router_group": nrm(ks[11], (DEPTH, N_EXPERT_GROUPS), 0.01),
        "w_router_expert": nrm(ks[12], (DEPTH, D_MODEL, N_EXPERTS), D_MODEL ** -0.5),
        "b_router_expert": nrm(ks[13], (DEPTH, N_EXPERTS), 0.01),
        "w_expert_gate": nrm(ks[14], (DEPTH, N_EXPERTS, D_MODEL, D_EXPERT), D_MODEL ** -0.5),
        "w_expert_up": nrm(ks[15], (DEPTH, N_EXPERTS, D_MODEL, D_EXPERT), D_MODEL ** -0.5),
        "w_expert_down": nrm(ks[16], (DEPTH, N_EXPERTS, D_EXPERT, D_MODEL), D_EXPERT ** -0.5),
    }


def reference(x, attn_norm_g, w_qkv, q_norm_g, k_norm_g, w_attn_out,
              fourier_norm_g, w_fourier_in, w_fourier_out,
              moe_norm_g, w_router_group, b_router_group, w_router_expert, b_router_expert,
              w_expert_gate, w_expert_up, w_expert_down):
    for i in range(DEPTH):
        j = i // N_MIXERS
        if i % N_MIXERS == 0:
            x = x + dilated_attention_mixer(x, attn_norm_g[j], w_qkv[j], q_norm_g[j], k_norm_g[j], w_attn_out[j])
        else:
            x = x + fourier_mixer(x, fourier_norm_g[j], w_fourier_in[j], w_fourier_out[j])
        x = x + hierarchical_moe(x, moe_norm_g[i], w_router_group[i], b_router_group[i],
                                 w_router_expert[i], b_router_expert[i],
                                 w_expert_gate[i], w_expert_up[i], w_expert_down[i])
    return x
```

```python
import math
from contextlib import ExitStack
import numpy as np
import ml_dtypes
import concourse.bass as bass
import concourse.mybir as mybir
from concourse.bass_utils import run_bass_kernel_spmd

F32 = mybir.dt.float32
BF16 = mybir.dt.bfloat16
I32 = mybir.dt.int32
U32 = mybir.dt.uint32
ALU = mybir.AluOpType
AF = mybir.ActivationFunctionType
AX = mybir.AxisListType

S = 2048
D = 2048
NT = S // 128
NK = D // 128
NH = 12
HD = 128
AW = NH * HD
NE = 64
DE = 512
CAP = 128
EPS = 1e-6
SHIFT = 6.0


_UID = [0]


def U(name):
    _UID[0] += 1
    return f"{name}_{_UID[0]}"


class T:
    def __init__(self, name, ap_fn=None):
        self.name = U(name)
        self.ap_fn = ap_fn
        self.w = {}
        self.r = {}
        self.dsem = None
        self.dcount = 0


class Ctx:
    def __init__(self, nc, stack):
        self.nc = nc
        self.stack = stack
        self.eng = {"pe": nc.tensor, "act": nc.scalar, "dve": nc.vector, "pool": nc.gpsimd, "sp": nc.sync}
        self.sem = {k: stack.enter_context(nc.semaphore("prog_" + k)) for k in self.eng}
        self.cnt = {k: 0 for k in self.eng}
        self.waited = {k: {} for k in self.eng}
        self.semobj = {("e", k): self.sem[k] for k in self.eng}
        self.dma_sems = []
        self.log = {k: [] for k in self.eng}

    def _wait(self, e, key, val):
        if val <= 0:
            return
        if key == ("e", e) and e == "pe":
            return
        if self.waited[e].get(key, 0) >= val:
            return
        if key[0] == "e":
            assert self.cnt[key[1]] >= val, f"wait on unsignaled {key} {val} > {self.cnt[key[1]]}"
        self.eng[e].wait_ge(self.semobj[key], val)
        self.waited[e][key] = val
        self.log[e].append(("w", key, val))

    def _deps(self, e, reads, writes):
        for t in reads:
            for k, v in t.w.items():
                self._wait(e, k, v)
        for t in writes:
            for k, v in t.w.items():
                self._wait(e, k, v)
            for k, v in t.r.items():
                if k == ("e", e):
                    continue
                self._wait(e, k, v)

    def op(self, e, fn, reads=(), writes=(), signal=True):
        self._deps(e, reads, writes)
        inst = fn()
        if signal:
            self.cnt[e] += 1
            inst.then_inc(self.sem[e], 1)
            seq = self.cnt[e]
            self.log[e].append(("s", ("e", e), 1))
        else:
            seq = self.cnt[e] + 1
        key = ("e", e)
        for t in reads:
            t.r[key] = max(t.r.get(key, 0), seq)
        for t in writes:
            t.r = {}
        for t in writes:
            t.w[key] = max(t.w.get(key, 0), seq)
        return inst

    def dma(self, q, out_ap, in_ap, reads=(), writes=(), dst=None, **kw):
        assert dst is not None
        if dst.dsem is None:
            dst.dsem = self.stack.enter_context(self.nc.semaphore("d_" + dst.name))
            self.semobj[("d", dst.name)] = dst.dsem
            self.dma_sems.append(dst)
        self._deps(q, reads, writes)
        inst = self.eng[q].dma_start(out=out_ap, in_=in_ap, **kw)
        dst.dcount += 16
        inst.then_inc(dst.dsem, 16)
        self.log[q].append(("s", ("d", dst.name), 16))
        key = ("d", dst.name)
        for t in reads:
            t.r[key] = max(t.r.get(key, 0), dst.dcount)
        for t in writes:
            t.r = {}
        for t in writes:
            t.w[key] = max(t.w.get(key, 0), dst.dcount)
        return inst

    def gather(self, out_ap, in_ap, idx_ap, reads=(), writes=(), dst=None):
        if dst.dsem is None:
            dst.dsem = self.stack.enter_context(self.nc.semaphore("d_" + dst.name))
            self.semobj[("d", dst.name)] = dst.dsem
            self.dma_sems.append(dst)
        self._deps("pool", reads, writes)
        inst = self.nc.gpsimd.indirect_dma_start(
            out=out_ap, out_offset=None, in_=in_ap,
            in_offset=bass.IndirectOffsetOnAxis(ap=idx_ap, axis=0))
        dst.dcount += 16
        inst.then_inc(dst.dsem, 16)
        self.log["pool"].append(("s", ("d", dst.name), 16))
        key = ("d", dst.name)
        for t in reads:
            t.r[key] = max(t.r.get(key, 0), dst.dcount)
        for t in writes:
            t.r = {}
        for t in writes:
            t.w[key] = max(t.w.get(key, 0), dst.dcount)
        return inst

    def barrier(self):
        for e in self.eng:
            for f in self.eng:
                if f != e:
                    self._wait(e, ("e", f), self.cnt[f])
            for t in self.dma_sems:
                self._wait(e, ("d", t.name), t.dcount)

    def final_wait(self, ts):
        for t in ts:
            self._wait("sp", ("d", t.name), t.dcount)


def make_consts():
    bf = ml_dtypes.bfloat16
    c = {}
    c["identb"] = np.eye(128, dtype=np.float32).astype(bf)
    c["identf"] = np.eye(128, dtype=np.float32)
    half = 16
    inv_freq = (500000.0 ** (-np.arange(0, 32, 2, dtype=np.float32) / 32)).astype(np.float32)
    ang = np.arange(S, dtype=np.float32)[:, None] * inv_freq[None, :]
    cosf = np.ones((128, S), np.float32)
    sinf = np.zeros((128, S), np.float32)
    cosf[0:16] = np.cos(ang).T
    cosf[16:32] = np.cos(ang).T
    sinf[0:16] = np.sin(ang).T
    sinf[16:32] = np.sin(ang).T
    c["ropec"] = cosf
    c["ropes"] = sinf
    rot = np.zeros((128, 128), np.float32)
    for m in range(16):
        rot[m + 16, m] = -1.0
        rot[m, m + 16] = 1.0
    c["rotP"] = rot.astype(bf)
    a = np.arange(128)[:, None]
    b = np.arange(128)[None, :]
    mL = (a - b >= 64)
    mD = (np.abs(a - b) <= 64)
    mU = (b - a >= 64)
    c["bmask"] = np.concatenate([mL, mD, mU], axis=1).astype(np.float32).astype(bf)
    ch = np.arange(256)
    angc = 2 * np.pi * ((ch[:, None] * ch[None, :]) % 256) / 256.0
    c["cs_c"] = np.concatenate([np.cos(angc) / 16.0, np.sin(angc) / 16.0], axis=1).astype(np.float32).astype(bf)
    t = np.arange(S, dtype=np.int64)
    angs = 2 * np.pi * ((t[:, None] * t[None, :]) % S) / float(S)
    sc = 1.0 / math.sqrt(S)
    c["dft_c"] = (np.cos(angs) * sc).astype(np.float32).astype(bf)
    c["dft_s"] = (-np.sin(angs) * sc).astype(np.float32).astype(bf)
    c["tri"] = (a <= b).astype(np.float32).astype(bf)
    c["iota_c"] = np.broadcast_to(np.arange(128, dtype=np.float32)[None, :], (128, 128)).copy()
    c["iota_e"] = np.broadcast_to((128.0 * np.arange(64, dtype=np.float32))[None, :], (128, 64)).copy()
    c["iota_cb"] = c["iota_c"].astype(bf)
    return c


CONST_SPECS = {
    "identb": ([128, 128], BF16), "identf": ([128, 128], F32), "ropec": ([128, S], F32), "ropes": ([128, S], F32),
    "rotP": ([128, 128], BF16), "bmask": ([128, 384], BF16), "cs_c": ([256, 512], BF16),
    "dft_c": ([S, S], BF16), "dft_s": ([S, S], BF16), "tri": ([128, 128], BF16),
    "iota_c": ([128, 128], F32), "iota_e": ([128, 64], F32), "iota_cb": ([128, 128], BF16),
}

IN_SPECS = {
    "x": [S, D], "attn_norm_g": [1, D], "w_qkv": [D, 3 * AW], "q_norm_g": [1, HD], "k_norm_g": [1, HD],
    "w_attn_out": [AW, D], "fourier_norm_g": [1, D], "w_fourier_in": [D, D], "w_fourier_out": [D, D],
    "moe_norm_g": [2, D], "w_router_group": [2, D, 8], "b_router_group": [2, 8],
    "w_router_expert": [2, D, 64], "b_router_expert": [2, 64],
    "w_expert_gate": [2, NE, D, DE], "w_expert_up": [2, NE, D, DE], "w_expert_down": [2, NE, DE, D],
}


class G:
    pass


def phase_view(ap2d, d):
    return ap2d.rearrange("p (j r) -> p r j", r=d)


def blk512(ap2d, d, n):
    L = S // d
    v = phase_view(ap2d, d)
    if L >= 512:
        r = (512 * n) // L
        j0 = (512 * n) % L
        return v[:, r, j0:j0 + 512], False
    npb = 512 // L
    return v[:, n * npb:(n + 1) * npb, :], True


def blk128(ap2d, d, c):
    L = S // d
    v = phase_view(ap2d, d)
    r = (128 * c) // L
    j0 = (128 * c) % L
    return v[:, r, j0:j0 + 128]


def norm_to_hT(g, st, x_src, gvec_ap, hT, ThT):
    nc, cx = g.nc, g.cx
    with ExitStack() as s2:
        sb = lambda n, s, d: s2.enter_context(nc.sbuf_tensor(U(n), s, d))
        gb = sb("n_gb", [128, D], F32); Tgb = T("n_gb")
        junk = sb("n_junk", [128, D], BF16); Tjunk = T("n_junk")
        xts = [sb(f"n_xt{i}", [128, D], F32) for i in range(2)]; Txt = [T(f"n_xt{i}") for i in range(2)]
        hbs = [sb(f"n_hb{i}", [128, D], BF16) for i in range(2)]; Thb = [T(f"n_hb{i}") for i in range(2)]
        ss = sb("n_ss", [128, 2], F32); Tss = [T("n_ss0"), T("n_ss1")]
        rs = sb("n_rs", [128, 2], F32); Trs = [T("n_rs0"), T("n_rs1")]
        pts = [s2.enter_context(nc.psum_tensor(U(f"n_pt{i}"), [128, 1024], BF16)) for i in range(2)]
        Tpt = [T(f"n_pt{i}") for i in range(2)]
        cx.dma("sp", gb[:, :], gvec_ap.broadcast_to([128, D]), writes=[Tgb], dst=Tgb)

        def front(i):
            b = i % 2
            xt, hb = xts[b], hbs[b]
            cx.dma("sp", xt[:, :], x_src[i * 128:(i + 1) * 128, :], writes=[Txt[b]], dst=Txt[b])
            cx.op("act", lambda: nc.scalar.activation(out=junk[:, :], in_=xt[:, :], func=AF.Square, accum_out=ss[:, b:b + 1]),
                  reads=[Txt[b]], writes=[Tjunk, Tss[b]])
            cx.op("act", lambda: nc.scalar.activation(out=rs[:, b:b + 1], in_=ss[:, b:b + 1], func=AF.Sqrt, scale=1.0 / D, bias=EPS),
                  reads=[Tss[b]], writes=[Trs[b]])
            cx.op("dve", lambda: nc.vector.reciprocal(out=rs[:, b:b + 1], in_=rs[:, b:b + 1]), reads=[Trs[b]], writes=[Trs[b]])
            cx.op("dve", lambda: nc.vector.scalar_tensor_tensor(out=hb[:, :], in0=xt[:, :], scalar=rs[:, b:b + 1], in1=gb[:, :],
                                                                op0=ALU.mult, op1=ALU.mult),
                  reads=[Txt[b], Trs[b], Tgb], writes=[Thb[b]])

        def back(i):
            b = i % 2
            hb = hbs[b]
            for half in range(2):
                pt = pts[half]
                for c in range(8):
                    k = half * 8 + c
                    cx.op("pe", lambda: nc.tensor.transpose(out=pt[:, c * 128:(c + 1) * 128], in_=hb[:, k * 128:(k + 1) * 128],
                                                            identity=g.identb[:, :]),
                          reads=[Thb[b], g.Tconst], writes=[Tpt[half]], signal=(c == 7))
                if half == 0:
                    cx.op("act", lambda: nc.scalar.copy(out=hT[:, half * 8:(half + 1) * 8, i * 128:(i + 1) * 128],
                                                        in_=pt[:, :].rearrange("p (c t) -> p c t", c=8)),
                          reads=[Tpt[half]], writes=[ThT])
                else:
                    cx.op("dve", lambda: nc.vector.tensor_copy(out=hT[:, half * 8:(half + 1) * 8, i * 128:(i + 1) * 128],
                                                               in_=pt[:, :].rearrange("p (c t) -> p c t", c=8)),
                          reads=[Tpt[half]], writes=[ThT])

        front(0)
        for i in range(NT):
            if i + 1 < NT:
                front(i + 1)
            back(i)
        cx.barrier()


def cast_piece(g, idx, out_ap, in_ap, Tin, Tout):
    nc, cx = g.nc, g.cx
    e = ("act", "dve")[idx % 2]
    if e == "act":
        cx.op("act", lambda: nc.scalar.copy(out=out_ap, in_=in_ap), reads=[Tin], writes=[Tout])
    else:
        cx.op("dve", lambda: nc.vector.tensor_copy(out=out_ap, in_=in_ap), reads=[Tin], writes=[Tout])


def phase_attn(g, x_src, mixT_dram, Tmix):
    nc, cx = g.nc, g.cx
    dram = g.dram
    DIL = (1, 4, 16)
    with ExitStack() as st:
        sb = lambda n, s, d: st.enter_context(nc.sbuf_tensor(U(n), s, d))
        hT = sb("a_hT", [128, NK, S], BF16); ThT = T("a_hT")
        norm_to_hT(g, st, x_src, dram["attn_norm_g"][0:1, :], hT, ThT)
        ropec = sb("a_ropec", [128, S], F32); ropes = sb("a_ropes", [128, S], F32); Trope = T("a_rope")
        qgc = sb("a_qg", [128, 2], F32); Tqg = T("a_qg")
        wst = [sb(f"a_wst{j}", [128, NK, 128], F32) for j in range(3)]; Twst = [T(f"a_wst{j}") for j in range(3)]
        wbf = [[sb(f"a_wbf{b}_{j}", [128, NK, 128], BF16) for j in range(3)] for b in range(2)]
        Twbf = [[T(f"a_wbf{b}_{j}") for j in range(3)] for b in range(2)]
        qk = [sb(f"a_qk{j}", [128, S], BF16) for j in range(2)]; Tqk = [T(f"a_qk{j}") for j in range(2)]
        vsb = sb("a_v", [128, NT, 128], BF16); Tv = T("a_v")
        sq = [sb(f"a_sq{i}", [128, 512], F32) for i in range(2)]; Tsq = [T(f"a_sq{i}") for i in range(2)]
        rq = [sb(f"a_rq{i}", [128, 512], F32) for i in range(2)]; Trq = [T(f"a_rq{i}") for i in range(2)]
        qgb = [sb(f"a_qgb{i}", [128, 512], BF16) for i in range(2)]; Tqgb = [T(f"a_qgb{i}") for i in range(2)]
        t1 = [sb(f"a_t1{i}", [128, 512], F32) for i in range(2)]; Tt1 = [T(f"a_t1{i}") for i in range(2)]
        t2 = [sb(f"a_t2{i}", [128, 512], F32) for i in range(2)]; Tt2 = [T(f"a_t2{i}") for i in range(2)]
        pTs = [sb(f"a_pT{j}", [128, 384], BF16) for j in range(2)]; TpT = [T(f"a_pT{j}") for j in range(2)]
        onat = sb("a_onat", [128, 3, S], BF16); Tonat = [T(f"a_onat{j}") for j in range(3)]
        Ltot = sb("a_L", [128, S], F32); TL = T("a_L")
        mixo = sb("a_mixo", [128, 3, S], BF16); Tmixo = [T(f"a_mixo{j}") for j in range(3)]
        onesf = sb("a_onesf", [128, 128], F32); onesb = sb("a_onesb", [128, 128], BF16); Tones = T("a_ones")
        bank = [st.enter_context(nc.psum_tensor(U(f"a_ps{j}"), [128, 512], F32)) for j in range(8)]
        Tb = [T(f"a_ps{j}") for j in range(8)]
        PQ0, PQ1, PSS, PROT, PV, PST, PO, PL = range(8)

        cx.dma("sp", ropec[:, :], dram["ropec"][:, :], writes=[Trope], dst=Trope)
        cx.dma("sp", ropes[:, :], dram["ropes"][:, :], writes=[Trope], dst=Trope)
        with nc.allow_non_contiguous_dma(reason="tiny gain columns"):
            cx.dma("sp", qgc[:, 0:1], dram["q_norm_g"].rearrange("o d -> d o"), writes=[Tqg], dst=Tqg)
            cx.dma("sp", qgc[:, 1:2], dram["k_norm_g"].rearrange("o d -> d o"), writes=[Tqg], dst=Tqg)
        cx.op("dve", lambda: nc.vector.memset(onesf[:, :], 1.0), writes=[Tones])
        cx.op("dve", lambda: nc.vector.memset(onesb[:, :], 1.0), writes=[Tones])

        wq = dram["w_qkv"]
        heads = [(hg, gi) for hg in range(4) for gi in range(3)]

        def load_w(hidx):
            hg, gi = heads[hidx]
            h = gi * 4 + hg
            for j in range(3):
                cx.dma("sp", wst[j][:, :, :], wq[:, j * AW + h * 128: j * AW + (h + 1) * 128].rearrange("(k p) n -> p k n", p=128),
                       writes=[Twst[j]], dst=Twst[j])

        def cast_w(hidx):
            b = hidx % 2
            for j in range(3):
                cast_piece(g, j, wbf[b][j][:, :, :], wst[j][:, :, :], Twst[j], Twbf[b][j])

        load_w(0)
        cast_w(0)
        for hidx, (hg, gi) in enumerate(heads):
            d = DIL[gi]
            L = S // d
            bps = L // 128
            wb = wbf[hidx % 2]
            Twb = Twbf[hidx % 2]
            if hidx + 1 < len(heads):
                load_w(hidx + 1)
            blocks = [(j, n) for j in range(2) for n in range(4)]

            def qk_front(bi):
                j, n = blocks[bi]
                p = bi % 2
                pq = bank[PQ0 + p]; Tpq = Tb[PQ0 + p]
                for k in range(NK):
                    cx.op("pe", lambda: nc.tensor.matmul(out=pq[:, :], lhsT=wb[j][:, k, :], rhs=hT[:, k, n * 512:(n + 1) * 512], start=(k == 0), stop=(k == NK - 1)),
                          reads=[ThT, Twb[j]], writes=[Tpq], signal=(k == NK - 1))
                cx.op("act", lambda: nc.scalar.activation(out=sq[p][:, :], in_=pq[:, :], func=AF.Square), reads=[Tpq], writes=[Tsq[p]])
                cx.op("act", lambda: nc.scalar.mul(out=qgb[p][:, :], in_=pq[:, :], mul=qgc[:, j:j + 1]), reads=[Tpq, Tqg], writes=[Tqgb[p]])

            def qk_back(bi):
                j, n = blocks[bi]
                p = bi % 2
                pss = bank[(PSS, PST)[p]]; Tpss = Tb[(PSS, PST)[p]]
                prot = bank[(PROT, PV)[p]]; Tprot = Tb[(PROT, PV)[p]]
                cx.op("pe", lambda: nc.tensor.matmul(out=pss[:, :], lhsT=onesf[:, :], rhs=sq[p][:, :], start=True, stop=True),
                      reads=[Tsq[p], Tones], writes=[Tpss])
                cx.op("pe", lambda: nc.tensor.matmul(out=prot[:, :], lhsT=g.rotP[:, :], rhs=qgb[p][:, :], start=True, stop=True),
                      reads=[Tqgb[p], g.Tconst], writes=[Tprot])
                cx.op("act", lambda: nc.scalar.activation(out=rq[p][:, :], in_=pss[:, :], func=AF.Ln, scale=1.0 / HD, bias=EPS),
                      reads=[Tpss], writes=[Trq[p]])
                cx.op("act", lambda: nc.scalar.activation(out=rq[p][:, :], in_=rq[p][:, :], func=AF.Exp, scale=-0.5), reads=[Trq[p]], writes=[Trq[p]])
                cx.op("dve", lambda: nc.vector.tensor_tensor(out=t1[p][:, :], in0=qgb[p][:, :], in1=ropec[:, n * 512:(n + 1) * 512], op=ALU.mult),
                      reads=[Tqgb[p], Trope], writes=[Tt1[p]])
                cx.op("dve", lambda: nc.vector.tensor_tensor(out=t2[p][:, :], in0=prot[:, :], in1=ropes[:, n * 512:(n + 1) * 512], op=ALU.mult),
                      reads=[Tprot, Trope], writes=[Tt2[p]])
                cx.op("pool", lambda: nc.gpsimd.tensor_tensor(out=t1[p][:, :], in0=t1[p][:, :], in1=t2[p][:, :], op=ALU.add),
                      reads=[Tt1[p], Tt2[p]], writes=[Tt1[p]])
                if d == 1:
                    oap = qk[j][:, n * 512:(n + 1) * 512]
                    i0 = t1[p][:, :]; i1 = rq[p][:, :]
                else:
                    w_ = 512 // d
                    oap = qk[j][:, :].rearrange("p (r j) -> p r j", r=d)[:, :, n * w_:(n + 1) * w_]
                    i0 = t1[p][:, :].rearrange("p (jj r) -> p r jj", r=d)
                    i1 = rq[p][:, :].rearrange("p (jj r) -> p r jj", r=d)
                cx.op("dve", lambda: nc.vector.tensor_tensor(out=oap, in0=i0, in1=i1, op=ALU.mult),
                      reads=[Tt1[p], Trq[p]], writes=[Tqk[j]])

            for bi in range(len(blocks) + 1):
                if bi < len(blocks):
                    qk_front(bi)
                if bi >= 1:
                    qk_back(bi - 1)
            for c4 in range(4):
                for cc in range(4):
                    c = c4 * 4 + cc
                    for k in range(NK):
                        cx.op("pe", lambda: nc.tensor.matmul(out=bank[PV][:, cc * 128:(cc + 1) * 128], lhsT=blk128(hT[:, k, :], d, c),
                                                             rhs=wb[2][:, k, :], start=(k == 0), stop=(k == NK - 1)),
                              reads=[ThT, Twb[2]], writes=[Tb[PV]], signal=(k == NK - 1 and cc == 3))
                cx.op("act", lambda: nc.scalar.copy(out=vsb[:, c4 * 4:(c4 + 1) * 4, :], in_=bank[PV][:, :].rearrange("p (a b) -> p a b", b=128)),
                      reads=[Tb[PV]], writes=[Tv])
            if hidx + 1 < len(heads):
                cast_w(hidx + 1)
            r3 = (lambda ap: ap.rearrange("p (a b) -> p a b", b=L)) if L < 512 else (lambda ap: ap)

            def keychunks(i):
                return [c for c in (i - 1, i, i + 1) if 0 <= c < NT and c // bps == i // bps]

            def att_front(i):
                cs = keychunks(i)
                lo = cs[0] - (i - 1)
                nc_ = len(cs)
                pT = pTs[i % 2]; TpTi = TpT[i % 2]
                pst = bank[(PST, PSS)[i % 2]]; Tpst = Tb[(PST, PSS)[i % 2]]
                for jj, c in enumerate(cs):
                    cx.op("pe", lambda: nc.tensor.matmul(out=pst[:, jj * 128:(jj + 1) * 128], lhsT=qk[1][:, c * 128:(c + 1) * 128],
                                                         rhs=qk[0][:, i * 128:(i + 1) * 128], start=True, stop=True),
                          reads=[Tqk[0], Tqk[1]], writes=[Tpst], signal=(jj == nc_ - 1))
                cx.op("act", lambda: nc.scalar.activation(out=pT[:, 0:nc_ * 128], in_=pst[:, 0:nc_ * 128], func=AF.Exp,
                                                          scale=float(HD) ** -0.5, bias=-SHIFT),
                      reads=[Tpst], writes=[TpTi])
                cx.op("dve", lambda: nc.vector.tensor_tensor(out=pT[:, 0:nc_ * 128], in0=pT[:, 0:nc_ * 128],
                                                             in1=g.bmask[:, lo * 128:(lo + nc_) * 128], op=ALU.mult),
                      reads=[TpTi, g.Tconst], writes=[TpTi])

            def att_back(i):
                cs = keychunks(i)
                nc_ = len(cs)
                n, ii = i // 4, i % 4
                pT = pTs[i % 2]; TpTi = TpT[i % 2]
                po = bank[(PO, PROT)[n % 2]]; Tpo = Tb[(PO, PROT)[n % 2]]
                pl = bank[(PL, PV)[n % 2]]; Tpl = Tb[(PL, PV)[n % 2]]
                for jj, c in enumerate(cs):
                    cx.op("pe", lambda: nc.tensor.matmul(out=po[:, ii * 128:(ii + 1) * 128], lhsT=vsb[:, c, :], rhs=pT[:, jj * 128:(jj + 1) * 128],
                                                         start=(jj == 0), stop=(jj == nc_ - 1)),
                          reads=[Tv, TpTi], writes=[Tpo], signal=False)
                for jj, c in enumerate(cs):
                    cx.op("pe", lambda: nc.tensor.matmul(out=pl[:, ii * 128:(ii + 1) * 128], lhsT=onesb[:, :], rhs=pT[:, jj * 128:(jj + 1) * 128],
                                                         start=(jj == 0), stop=(jj == nc_ - 1)),
                          reads=[Tones, TpTi], writes=[Tpl], signal=(jj == nc_ - 1))
                if ii == 3:
                    oview, _ = blk512(onat[:, gi, :], d, n)
                    lview, _ = blk512(Ltot[:, :], d, n)
                    cx.op("act", lambda: nc.scalar.copy(out=oview, in_=r3(po[:, :])), reads=[Tpo], writes=[Tonat[gi]])
                    if gi == 0:
                        cx.op("dve", lambda: nc.vector.tensor_copy(out=lview, in_=r3(pl[:, :])), reads=[Tpl], writes=[TL])
                    else:
                        cx.op("dve", lambda: nc.vector.tensor_tensor(out=lview, in0=r3(pl[:, :]), in1=lview, op=ALU.add),
                              reads=[Tpl, TL], writes=[TL])

            for i in range(NT + 1):
                if i < NT:
                    att_front(i)
                if i >= 1:
                    att_back(i - 1)
            if gi == 2:
                cx.op("act", lambda: nc.scalar.activation(out=Ltot[:, :], in_=Ltot[:, :], func=AF.Ln), reads=[TL], writes=[TL])
                cx.op("act", lambda: nc.scalar.activation(out=Ltot[:, :], in_=Ltot[:, :], func=AF.Exp, scale=-1.0), reads=[TL], writes=[TL])
                for g2 in range(3):
                    e = ("dve", "pool", "dve")[g2]
                    eng = nc.vector if e == "dve" else nc.gpsimd
                    cx.op(e, lambda: eng.tensor_tensor(out=mixo[:, g2, :], in0=onat[:, g2, :], in1=Ltot[:, :], op=ALU.mult),
                          reads=[Tonat[g2], TL], writes=[Tmixo[g2]])
                    h = g2 * 4 + hg
                    cx.dma("sp", mixT_dram[h * 128:(h + 1) * 128, :], mixo[:, g2, :], reads=[Tmixo[g2]], writes=[Tmix], dst=Tmix)
        cx.barrier()


def phase_outproj(g, srcT_dram, Tsrc, KC, w_dram, x_old, Told, x_new, Tnew):
    nc, cx = g.nc, g.cx
    with ExitStack() as st:
        sb = lambda n, s, d: st.enter_context(nc.sbuf_tensor(U(n), s, d))
        sT = sb("o_sT", [128, KC, S], BF16); TsT = T("o_sT")
        wst = [sb(f"o_wst{j}", [128, 4, 512], F32) for j in range(2)]; Twst = [T(f"o_wst{j}") for j in range(2)]
        wbf = [sb(f"o_wbf{j}", [128, KC, 512], BF16) for j in range(2)]; Twbf = [T(f"o_wbf{j}") for j in range(2)]
        xts = [sb(f"o_xt{j}", [128, 512], F32) for j in range(4)]; Txt = [T(f"o_xt{j}") for j in range(4)]
        bank = [st.enter_context(nc.psum_tensor(U(f"o_ps{j}"), [128, 512], F32)) for j in range(4)]
        Tb = [T(f"o_ps{j}") for j in range(4)]
        for k in range(KC):
            cx.dma("sp", sT[:, k, :], srcT_dram[k * 128:(k + 1) * 128, :], reads=[Tsrc], writes=[TsT], dst=TsT)
        wv = w_dram.rearrange("(k p) n -> p k n", p=128)
        npieces = KC // 4
        pc = 0

        def load_cast(nb):
            nonlocal pc
            for q in range(npieces):
                j = pc % 2
                pc += 1
                cx.dma("sp", wst[j][:, :, :], wv[:, q * 4:(q + 1) * 4, nb * 512:(nb + 1) * 512], writes=[Twst[j]], dst=Twst[j])
                cast_piece(g, pc, wbf[nb % 2][:, q * 4:(q + 1) * 4, :], wst[j][:, :, :], Twst[j], Twbf[nb % 2])

        load_cast(0)
        seq = [(nb, i) for nb in range(4) for i in range(NT)]

        def load_x(q):
            nb_, i_ = seq[q]
            cx.dma("sp", xts[q % 4][:, :], x_old[i_ * 128:(i_ + 1) * 128, nb_ * 512:(nb_ + 1) * 512], reads=[Told], writes=[Txt[q % 4]], dst=Txt[q % 4])

        load_x(0)
        load_x(1)
        it = 0
        for nb in range(4):
            if nb + 1 < 4:
                load_cast(nb + 1)
            for i in range(NT):
                pb = bank[it % 4]; Tpb = Tb[it % 4]
                xt = xts[it % 4]; Txi = Txt[it % 4]
                if it + 2 < len(seq):
                    load_x(it + 2)
                it += 1
                for k in range(KC):
                    cx.op("pe", lambda: nc.tensor.matmul(out=pb[:, :], lhsT=sT[:, k, i * 128:(i + 1) * 128], rhs=wbf[nb % 2][:, k, :],
                                                         start=(k == 0), stop=(k == KC - 1)),
                          reads=[TsT, Twbf[nb % 2]], writes=[Tpb], signal=(k == KC - 1))
                cx.op("dve", lambda: nc.vector.tensor_tensor(out=xt[:, :], in0=pb[:, :], in1=xt[:, :], op=ALU.add),
                      reads=[Tpb, Txi], writes=[Txi])
                cx.dma("sp", x_new[i * 128:(i + 1) * 128, nb * 512:(nb + 1) * 512], xt[:, :], reads=[Txi], writes=[Tnew], dst=Tnew)
        cx.barrier()


def phase_fourier(g, x_src, fT_dram, TfT):
    nc, cx = g.nc, g.cx
    dram = g.dram
    with ExitStack() as st:
        sb = lambda n, s, d: st.enter_context(nc.sbuf_tensor(U(n), s, d))
        hT = sb("f_hT", [128, NK, S], BF16); ThT = T("f_hT")
        norm_to_hT(g, st, x_src, dram["fourier_norm_g"][0:1, :], hT, ThT)
        wst = sb("f_wst", [128, NK, 256], F32); Twst = T("f_wst")
        wbf = [sb(f"f_wbf{j}", [128, NK, 256], BF16) for j in range(2)]; Twbf = [T(f"f_wbf{j}") for j in range(2)]
        csc = sb("f_csc", [128, 2, 512], BF16); Tcsc = T("f_csc")
        uT = sb("f_uT", [128, 2, S], BF16); TuT = T("f_uT")
        vsb = sb("f_v", [128, NT, 512], BF16); Tv = T("f_v")
        tabs = [[sb(f"f_tab{b}_{j}", [128, NT, 512], BF16) for j in range(2)] for b in range(2)]
        Ttab = [[T(f"f_tab{b}_{j}") for j in range(2)] for b in range(2)]
        fo = [sb(f"f_fo{j}", [128, 512], BF16) for j in range(2)]; Tfo = [T(f"f_fo{j}") for j in range(2)]
        bank = [st.enter_context(nc.psum_tensor(U(f"f_ps{j}"), [128, 512], F32)) for j in range(4)]
        Tb = [T(f"f_ps{j}") for j in range(4)]
        cx.dma("sp", csc[:, :, :], dram["cs_c"].rearrange("(m p) n -> p m n", p=128), writes=[Tcsc], dst=Tcsc)
        wv = dram["w_fourier_in"].rearrange("(k p) n -> p k n", p=128)
        dftv = [dram["dft_c"].rearrange("(i p) s -> p i s", p=128), dram["dft_s"].rearrange("(i p) s -> p i s", p=128)]

        def load_w(gi):
            cx.dma("sp", wst[:, :, :], wv[:, :, gi * 256:(gi + 1) * 256], writes=[Twst], dst=Twst)
            cx.op("act", lambda: nc.scalar.copy(out=wbf[gi % 2][:, 0:8, :], in_=wst[:, 0:8, :]), reads=[Twst], writes=[Twbf[gi % 2]])
            cx.op("dve", lambda: nc.vector.tensor_copy(out=wbf[gi % 2][:, 8:16, :], in_=wst[:, 8:16, :]), reads=[Twst], writes=[Twbf[gi % 2]])

        tcount = 0

        def load_tab(n):
            nonlocal tcount
            b = tcount % 2
            tcount += 1
            for j in range(2):
                cx.dma("sp", tabs[b][j][:, :, :], dftv[j][:, :, n * 512:(n + 1) * 512], writes=[Ttab[b][j]], dst=Ttab[b][j])
            return b

        load_w(0)
        it = 0
        for gi in range(8):
            if gi + 1 < 8:
                load_w(gi + 1)
            for m in range(2):
                for n in range(4):
                    pb = bank[it % 4]; Tpb = Tb[it % 4]; it += 1
                    for k in range(NK):
                        cx.op("pe", lambda: nc.tensor.matmul(out=pb[:, :], lhsT=wbf[gi % 2][:, k, m * 128:(m + 1) * 128], rhs=hT[:, k, n * 512:(n + 1) * 512],
                                                             start=(k == 0), stop=(k == NK - 1)),
                              reads=[ThT, Twbf[gi % 2]], writes=[Tpb], signal=(k == NK - 1))
                    e = ("act", "dve")[it % 2]
                    if e == "act":
                        cx.op("act", lambda: nc.scalar.copy(out=uT[:, m, n * 512:(n + 1) * 512], in_=pb[:, :]), reads=[Tpb], writes=[TuT])
                    else:
                        cx.op("dve", lambda: nc.vector.tensor_copy(out=uT[:, m, n * 512:(n + 1) * 512], in_=pb[:, :]), reads=[Tpb], writes=[TuT])
            for i in range(NT):
                pb = bank[it % 4]; Tpb = Tb[it % 4]; it += 1
                for m in range(2):
                    cx.op("pe", lambda: nc.tensor.matmul(out=pb[:, :], lhsT=uT[:, m, i * 128:(i + 1) * 128], rhs=csc[:, m, :], start=(m == 0), stop=(m == 1)),
                          reads=[TuT, Tcsc], writes=[Tpb], signal=(m == 1))
                e = ("act", "dve")[it % 2]
                if e == "act":
                    cx.op("act", lambda: nc.scalar.copy(out=vsb[:, i, :], in_=pb[:, :]), reads=[Tpb], writes=[Tv])
                else:
                    cx.op("dve", lambda: nc.vector.tensor_copy(out=vsb[:, i, :], in_=pb[:, :]), reads=[Tpb], writes=[Tv])
            for n in range(4):
                if gi == 0 and n == 0:
                    nxt_b = load_tab(0)
                b = nxt_b
                if not (gi == 7 and n == 3):
                    nxt_b = load_tab((n + 1) % 4)
                for m in range(2):
                    pb = bank[it % 4]; Tpb = Tb[it % 4]; it += 1
                    for i in range(NT):
                        for j in range(2):
                            cx.op("pe", lambda: nc.tensor.matmul(out=pb[:, :], lhsT=vsb[:, i, j * 256 + m * 128: j * 256 + (m + 1) * 128],
                                                                 rhs=tabs[b][j][:, i, :], start=(i == 0 and j == 0), stop=(i == NT - 1 and j == 1)),
                                  reads=[Tv, Ttab[b][j]], writes=[Tpb], signal=(i == NT - 1 and j == 1))
                    f = fo[it % 2]; Tf = Tfo[it % 2]
                    e = ("act", "dve")[it % 2]
                    if e == "act":
                        cx.op("act", lambda: nc.scalar.copy(out=f[:, :], in_=pb[:, :]), reads=[Tpb], writes=[Tf])
                    else:
                        cx.op("dve", lambda: nc.vector.tensor_copy(out=f[:, :], in_=pb[:, :]), reads=[Tpb], writes=[Tf])
                    row = gi * 256 + m * 128
                    cx.dma("sp", fT_dram[row:row + 128, n * 512:(n + 1) * 512], f[:, :], reads=[Tf], writes=[TfT], dst=TfT)
        cx.barrier()


def phase_moe(g, layer, x_src, Tsrc, x_dst, Tdst):
    nc, cx = g.nc, g.cx
    dram = g.dram
    hs, Ths, ys, Tys = g.hs, g.Ths, g.ys, g.Tys
    BIG = 1.0e30
    with ExitStack() as st:
        sb = lambda n, s, d: st.enter_context(nc.sbuf_tensor(U(n), s, d))
        maskall = sb("m_mask", [128, NT, NE], BF16); Tmask = T("m_mask")
        cw = sb("m_cw", [128, NT, 2], F32); Tcw = T("m_cw")
        sidf = sb("m_sidf", [128, NT, 2], F32); Tsidf = T("m_sidf")
        sidx = sb("m_sidx", [128, NT, 2], I32); Tsidx = T("m_sidx")
        tok = sb("m_tok", [128, NE], I32); Ttok = T("m_tok")
        onesb = sb("m_onesb", [128, 128], BF16); Tones = T("m_ones")
        cx.op("dve", lambda: nc.vector.memset(onesb[:, :], 1.0), writes=[Tones])
        NSLOT = 4
        stg = [sb(f"mc_stg{i}", [128, 8192], F32) for i in range(NSLOT)]; Tstg = [T(f"mc_stg{i}") for i in range(NSLOT)]
        weg = dram["w_expert_gate"]; weu = dram["w_expert_up"]; wed = dram["w_expert_down"]
        wsrc = [lambda e: weg[layer, e].rearrange("(p k) n -> p (k n)", p=128),
                lambda e: weu[layer, e].rearrange("(p k) n -> p (k n)", p=128),
                lambda e: wed[layer, e].rearrange("(p k) n -> p (k n)", p=128)]

        def load_w(e, j):
            q = (3 * e + j) % NSLOT
            cx.dma("sp", stg[q][:, :], wsrc[j](e), writes=[Tstg[q]], dst=Tstg[q])

        for q0 in range(NSLOT):
            load_w(q0 // 3, q0 % 3)
        sAB = ExitStack()
        m12 = sAB.enter_context(nc.sbuf_tensor(U("m_m12"), [128, NT, 2, NE], F32)); Tm12 = T("m_m12")
        cum = sAB.enter_context(nc.sbuf_tensor(U("m_cum"), [128, NT, NE], F32)); Tcum = T("m_cum")

        with ExitStack() as s2:
            sb2 = lambda n, s, d: s2.enter_context(nc.sbuf_tensor(U(n), s, d))
            gb = sb2("ma_gb", [128, D], F32); Tgb = T("ma_gb")
            gcol = sb2("ma_gcol", [128, NK], F32); Tgcol = T("ma_gcol")
            wr = sb2("ma_wr", [128, NK, 72], F32); Twr = T("ma_wr")
            bias = sb2("ma_bias", [128, 72], F32); Tbias = T("ma_bias")
            zrow = sb2("ma_zrow", [1, D], BF16); Tz = T("ma_zrow")
            xts = [sb2(f"ma_xt{i}", [128, D], F32) for i in range(2)]; Txt = [T(f"ma_xt{i}") for i in range(2)]
            hbs = [sb2(f"ma_hb{i}", [128, D], BF16) for i in range(2)]; Thb = [T(f"ma_hb{i}") for i in range(2)]
            xT = sb2("ma_xT", [128, NK, 128], F32); TxT = T("ma_xT")
            sms = [sb2(f"ma_sm{i}", [128, 4], F32) for i in range(2)]; Tsms = [T(f"ma_sm{i}") for i in range(2)]
            lgA = sb2("ma_lgA", [128, NT, 72], F32); TlgA = T("ma_lgA")
            r16 = sb2("ma_r16", [128, 8, NT], F32); Tr16 = T("ma_r16")
            gm = sb2("ma_gm", [128, NT, 8], F32); Tgm = T("ma_gm")
            ce = sb2("ma_ce", [128, NT, 8], F32); Tce = T("ma_ce")
            fm = sb2("ma_fm", [128, NT, NE], F32); Tfm = T("ma_fm")
            fm2 = fm; Tfm2 = Tfm
            pxt = [s2.enter_context(nc.psum_tensor(U(f"ma_px{i}"), [128, 512], F32)) for i in range(4)]
            Tpx = [T(f"ma_px{i}") for i in range(4)]
            plg = s2.enter_context(nc.psum_tensor(U("ma_plg"), [128, 72], F32)); Tplg = T("ma_plg")

            cx.dma("sp", gb[:, :], dram["moe_norm_g"][layer:layer + 1, :].broadcast_to([128, D]), writes=[Tgb], dst=Tgb)
            with nc.allow_non_contiguous_dma(reason="small router tables"):
                cx.dma("sp", gcol[:, :], dram["moe_norm_g"][layer].rearrange("(k p) -> p k", p=128), writes=[Tgcol], dst=Tgcol)
                cx.dma("sp", wr[:, :, 0:8], dram["w_router_group"][layer].rearrange("(k p) n -> p k n", p=128), writes=[Twr], dst=Twr)
                cx.dma("sp", wr[:, :, 8:72], dram["w_router_expert"][layer].rearrange("(k p) n -> p k n", p=128), writes=[Twr], dst=Twr)
                cx.dma("sp", bias[:, 0:8], dram["b_router_group"][layer:layer + 1, :].broadcast_to([128, 8]), writes=[Tbias], dst=Tbias)
                cx.dma("sp", bias[:, 8:72], dram["b_router_expert"][layer:layer + 1, :].broadcast_to([128, 64]), writes=[Tbias], dst=Tbias)
            cx.op("dve", lambda: nc.vector.memset(zrow[:, :], 0.0), writes=[Tz])
            cx.dma("sp", hs[S:S + 1, :], zrow[:, :], reads=[Tz], writes=[Ths], dst=Ths)
            for k in range(NK):
                cx.op("dve", lambda: nc.vector.tensor_scalar(out=wr[:, k, :], in0=wr[:, k, :], scalar1=gcol[:, k:k + 1], scalar2=None, op0=ALU.mult),
                      reads=[Twr, Tgcol], writes=[Twr])
            def a_front(i):
                b = i % 2
                xt, hb = xts[b], hbs[b]
                sm, Tsm = sms[b], Tsms[b]
                cx.dma("sp", xt[:, :], x_src[i * 128:(i + 1) * 128, :], reads=[Tsrc], writes=[Txt[b]], dst=Txt[b])
                cx.op("act", lambda: nc.scalar.activation(out=hb[:, :], in_=xt[:, :], func=AF.Square, accum_out=sm[:, 0:1]),
                      reads=[Txt[b]], writes=[Thb[b], Tsm])
                cx.op("act", lambda: nc.scalar.activation(out=sm[:, 1:2], in_=sm[:, 0:1], func=AF.Sqrt, scale=1.0 / D, bias=EPS),
                      reads=[Tsm], writes=[Tsm])
                cx.op("dve", lambda: nc.vector.reciprocal(out=sm[:, 1:2], in_=sm[:, 1:2]), reads=[Tsm], writes=[Tsm])
                cx.op("dve", lambda: nc.vector.scalar_tensor_tensor(out=hb[:, :], in0=xt[:, :], scalar=sm[:, 1:2], in1=gb[:, :],
                                                                    op0=ALU.mult, op1=ALU.mult),
                      reads=[Txt[b], Tsm, Tgb], writes=[Thb[b]])
                cx.dma("sp", hs[i * 128:(i + 1) * 128, :], hb[:, :], reads=[Thb[b]], writes=[Ths], dst=Ths)

            def a_back(i):
                b = i % 2
                xt = xts[b]
                sm, Tsm = sms[b], Tsms[b]
                for q in range(4):
                    for c in range(4):
                        k = q * 4 + c
                        cx.op("pe", lambda: nc.tensor.transpose(out=pxt[q][:, c * 128:(c + 1) * 128], in_=xt[:, k * 128:(k + 1) * 128], identity=g.identf[:, :]),
                              reads=[Txt[b], g.Tconst], writes=[Tpx[q]], signal=(c == 3))
                    if q % 2 == 0:
                        cx.op("act", lambda: nc.scalar.copy(out=xT[:, q * 4:(q + 1) * 4, :], in_=pxt[q][:, :].rearrange("p (c t) -> p c t", c=4)),
                              reads=[Tpx[q]], writes=[TxT])
                    else:
                        cx.op("dve", lambda: nc.vector.tensor_copy(out=xT[:, q * 4:(q + 1) * 4, :], in_=pxt[q][:, :].rearrange("p (c t) -> p c t", c=4)),
                              reads=[Tpx[q]], writes=[TxT])
                for k in range(NK):
                    cx.op("pe", lambda: nc.tensor.matmul(out=plg[:, :], lhsT=xT[:, k, :], rhs=wr[:, k, :], start=(k == 0), stop=(k == NK - 1)),
                          reads=[TxT, Twr], writes=[Tplg], signal=(k == NK - 1))
                cx.op("dve", lambda: nc.vector.scalar_tensor_tensor(out=lgA[:, i, :], in0=plg[:, :], scalar=sm[:, 1:2], in1=bias[:, :], op0=ALU.mult, op1=ALU.add),
                      reads=[Tplg, Tsm, Tbias], writes=[TlgA])

            for i in range(NT):
                a_front(i)
                a_back(i)
            V = nc.vector
            bc = lambda ap2, n: ap2.unsqueeze(2).broadcast_to([128, NT, n])
            coarse = lgA[:, :, 0:8]
            cx.op("dve", lambda: V.tensor_reduce(out=r16[:, 0, :], in_=coarse, axis=AX.X, op=ALU.max), reads=[TlgA], writes=[Tr16])
            cx.op("dve", lambda: V.tensor_tensor(out=gm[:, :, :], in0=coarse, in1=bc(r16[:, 0, :], 8), op=ALU.is_equal), reads=[TlgA, Tr16], writes=[Tgm])
            cx.op("dve", lambda: V.tensor_tensor(out=ce[:, :, :], in0=coarse, in1=bc(r16[:, 0, :], 8), op=ALU.subtract), reads=[TlgA, Tr16], writes=[Tce])
            cx.op("act", lambda: nc.scalar.activation(out=ce[:, :, :], in_=ce[:, :, :], func=AF.Exp), reads=[Tce], writes=[Tce])
            cx.op("dve", lambda: V.tensor_reduce(out=r16[:, 1, :], in_=ce[:, :, :], axis=AX.X, op=ALU.add), reads=[Tce], writes=[Tr16])
            cx.op("dve", lambda: V.reciprocal(out=r16[:, 2, :], in_=r16[:, 1, :]), reads=[Tr16], writes=[Tr16])
            cx.op("dve", lambda: V.tensor_scalar(out=gm[:, :, :], in0=gm[:, :, :], scalar1=BIG, scalar2=-BIG, op0=ALU.mult, op1=ALU.add),
                  reads=[Tgm], writes=[Tgm])
            cx.op("dve", lambda: V.tensor_tensor(out=fm[:, :, :].rearrange("p t (a b) -> p t a b", b=8),
                                                 in0=lgA[:, :, 8:72].rearrange("p t (a b) -> p t a b", b=8),
                                                 in1=gm[:, :, :].unsqueeze(3).broadcast_to([128, NT, 8, 8]), op=ALU.add),
                  reads=[TlgA, Tgm], writes=[Tfm])
            cx.op("dve", lambda: V.tensor_reduce(out=r16[:, 3, :], in_=fm[:, :, :], axis=AX.X, op=ALU.max), reads=[Tfm], writes=[Tr16])
            cx.op("dve", lambda: V.tensor_tensor(out=m12[:, :, 0, :], in0=fm[:, :, :], in1=bc(r16[:, 3, :], NE), op=ALU.is_equal),
                  reads=[Tfm, Tr16], writes=[Tm12])
            cx.op("dve", lambda: V.scalar_tensor_tensor(out=fm2[:, :, :], in0=m12[:, :, 0, :], scalar=-BIG, in1=fm[:, :, :], op0=ALU.mult, op1=ALU.add),
                  reads=[Tm12, Tfm], writes=[Tfm2])
            cx.op("dve", lambda: V.tensor_reduce(out=r16[:, 4, :], in_=fm2[:, :, :], axis=AX.X, op=ALU.max), reads=[Tfm2], writes=[Tr16])
            cx.op("dve", lambda: V.tensor_tensor(out=m12[:, :, 1, :], in0=fm2[:, :, :], in1=bc(r16[:, 4, :], NE), op=ALU.is_equal),
                  reads=[Tfm2, Tr16], writes=[Tm12])
            cx.op("dve", lambda: V.tensor_tensor(out=maskall[:, :, :], in0=m12[:, :, 0, :], in1=m12[:, :, 1, :], op=ALU.add), reads=[Tm12], writes=[Tmask])
            cx.op("dve", lambda: V.tensor_tensor(out=r16[:, 5, :], in0=r16[:, 4, :], in1=r16[:, 3, :], op=ALU.subtract), reads=[Tr16], writes=[Tr16])
            cx.op("act", lambda: nc.scalar.activation(out=r16[:, 5, :], in_=r16[:, 5, :], func=AF.Exp), reads=[Tr16], writes=[Tr16])
            cx.op("dve", lambda: V.tensor_scalar(out=r16[:, 6, :], in0=r16[:, 5, :], scalar1=1.0, scalar2=None, op0=ALU.add), reads=[Tr16], writes=[Tr16])
            cx.op("dve", lambda: V.reciprocal(out=r16[:, 6, :], in_=r16[:, 6, :]), reads=[Tr16], writes=[Tr16])
            cx.op("dve", lambda: V.tensor_tensor(out=cw[:, :, 0], in0=r16[:, 6, :], in1=r16[:, 2, :], op=ALU.mult), reads=[Tr16], writes=[Tcw])
            cx.op("dve", lambda: V.tensor_tensor(out=cw[:, :, 1], in0=cw[:, :, 0], in1=r16[:, 5, :], op=ALU.mult), reads=[Tr16, Tcw], writes=[Tcw])
            cx.barrier()

        with ExitStack() as s2:
            sb2 = lambda n, s, d: s2.enter_context(nc.sbuf_tensor(U(n), s, d))
            ind = [sb2(f"mb_ind{i}", [128, NE, CAP], BF16) for i in range(2)]; Tind = [T(f"mb_ind{i}") for i in range(2)]
            tmp = sb2("mb_tmp", [128, NT, NE], F32); Ttmp = T("mb_tmp")
            tmp2 = sb2("mb_tmp2", [128, NT, NE], F32); Ttmp2 = T("mb_tmp2")
            tokf = sb2("mb_tokf", [128, NE], F32); Ttokf = T("mb_tokf")
            cumb = sb2("mb_cumb", [128, NT, NE], BF16); Tcumb = T("mb_cumb")
            pcs = [s2.enter_context(nc.psum_tensor(U(f"mb_pc{i}"), [128, NE], F32)) for i in range(2)]; Tpc = [T(f"mb_pc{i}") for i in range(2)]
            ptok = s2.enter_context(nc.psum_tensor(U("mb_ptok"), [128, NE], F32)); Tptok = T("mb_ptok")
            V = nc.vector
            for i in range(NT):
                pc = pcs[i % 2]; Tp = Tpc[i % 2]
                for j in range(i):
                    cx.op("pe", lambda: nc.tensor.matmul(out=pc[:, :], lhsT=onesb[:, :], rhs=maskall[:, j, :], start=(j == 0), stop=False),
                          reads=[Tones, Tmask], writes=[Tp], signal=False)
                cx.op("pe", lambda: nc.tensor.matmul(out=pc[:, :], lhsT=g.tri[:, :], rhs=maskall[:, i, :], start=(i == 0), stop=True),
                      reads=[g.Tconst, Tmask], writes=[Tp])
                cx.op("act", lambda: nc.scalar.copy(out=cum[:, i, :], in_=pc[:, :]), reads=[Tp], writes=[Tcum])
                cx.op("act", lambda: nc.scalar.copy(out=cumb[:, i, :], in_=pc[:, :]), reads=[Tp], writes=[Tcumb])
            cx.op("dve", lambda: V.tensor_tensor(out=tmp[:, :, :], in0=cum[:, :, :], in1=maskall[:, :, :], op=ALU.subtract), reads=[Tcum, Tmask], writes=[Ttmp])
            cx.op("dve", lambda: V.tensor_tensor(out=tmp[:, :, :], in0=tmp[:, :, :], in1=g.iota_e[:, :].unsqueeze(1).broadcast_to([128, NT, NE]), op=ALU.add),
                  reads=[Ttmp, g.Tconst], writes=[Ttmp])
            for kk in range(2):
                cx.op("dve", lambda: V.tensor_tensor(out=tmp2[:, :, :], in0=tmp[:, :, :], in1=m12[:, :, kk, :], op=ALU.mult), reads=[Ttmp, Tm12], writes=[Ttmp2])
                cx.op("dve", lambda: V.tensor_reduce(out=sidf[:, :, kk], in_=tmp2[:, :, :], axis=AX.X, op=ALU.add), reads=[Ttmp2], writes=[Tsidf])
            for i in range(NT):
                idt = ind[i % 2]; Ti = Tind[i % 2]
                e_ = "dve"
                eng = nc.vector
                cx.op(e_, lambda: eng.tensor_tensor(out=idt[:, :, :], in0=cumb[:, i, :].unsqueeze(2).broadcast_to([128, NE, CAP]),
                                                    in1=g.iota_cb[:, :].unsqueeze(1).broadcast_to([128, NE, CAP]), op=ALU.is_le),
                      reads=[Tcumb, g.Tconst], writes=[Ti])
                for e in range(NE):
                    cx.op("pe", lambda: nc.tensor.matmul(out=ptok[:, e:e + 1], lhsT=idt[:, e, :], rhs=onesb[:, 0:1], start=True, stop=True),
                          reads=[Ti, Tones], writes=[Tptok], signal=(e == NE - 1))
                if i == 0:
                    cx.op("dve", lambda: V.tensor_copy(out=tokf[:, :], in_=ptok[:, :]), reads=[Tptok], writes=[Ttokf])
                else:
                    cx.op("dve", lambda: V.tensor_tensor(out=tokf[:, :], in0=ptok[:, :], in1=tokf[:, :], op=ALU.add), reads=[Tptok, Ttokf], writes=[Ttokf])
            cx.op("dve", lambda: V.tensor_copy(out=tok[:, :], in_=tokf[:, :]), reads=[Ttokf], writes=[Ttok])
            cx.op("dve", lambda: V.tensor_copy(out=sidx[:, :, :], in_=sidf[:, :, :]), reads=[Tsidf], writes=[Tsidx])
            if getattr(g, "dbg", None) is not None and layer == 0:
                Td = T("dbgout")
                cx.dma("sp", g.dbg["tok"][:, :], tok[:, :], reads=[Ttok], writes=[Td], dst=Td)
                cx.dma("sp", g.dbg["sidx"][:, :], sidx[:, :, :].rearrange("p a b -> p (a b)"), reads=[Tsidx], writes=[Td], dst=Td)
                cx.dma("sp", g.dbg["cum"][:, :], cum[:, :, :].rearrange("p a b -> p (a b)"), reads=[Tcum], writes=[Td], dst=Td)
                cx.dma("sp", g.dbg["cw"][:, :], cw[:, :, :].rearrange("p a b -> p (a b)"), reads=[Tcw], writes=[Td], dst=Td)
                cx.dma("sp", g.dbg["m12"][:, :], m12[:, :, :, :].rearrange("p a b c -> p (a b c)"), reads=[Tm12], writes=[Td], dst=Td)
            cx.barrier()

        sAB.close()
        with ExitStack() as s2:
            sb2 = lambda n, s, d: s2.enter_context(nc.sbuf_tensor(U(n), s, d))
            wg = sb2("mc_wg", [128, NK, DE], BF16); Twg = T("mc_wg")
            wu = sb2("mc_wu", [128, NK, DE], BF16); Twu = T("mc_wu")
            wd = sb2("mc_wd", [128, 4, D], BF16); Twd = T("mc_wd")
            xe = [sb2(f"mc_xe{i}", [128, D], BF16) for i in range(2)]; Txe = [T(f"mc_xe{i}") for i in range(2)]
            xeT = sb2("mc_xeT", [128, NK, 128], BF16); TxeT = T("mc_xeT")
            asb = sb2("mc_a", [128, DE], F32); Ta = T("mc_a")
            abf = sb2("mc_abf", [128, DE], BF16); Tabf = T("mc_abf")
            aT = sb2("mc_aT", [128, 4, 128], BF16); TaT = T("mc_aT")
            ysb = [sb2(f"mc_y{i}", [128, D], BF16) for i in range(2)]; Tysb = [T(f"mc_y{i}") for i in range(2)]
            ptr = [s2.enter_context(nc.psum_tensor(U(f"mc_ptr{i}"), [128, 1024], BF16)) for i in range(2)]; Tptr = [T(f"mc_ptr{i}") for i in range(2)]
            pg = s2.enter_context(nc.psum_tensor(U("mc_pg"), [128, 512], F32)); Tpg = T("mc_pg")
            pu = s2.enter_context(nc.psum_tensor(U("mc_pu"), [128, 512], F32)); Tpu = T("mc_pu")
            py = [s2.enter_context(nc.psum_tensor(U(f"mc_py{i}"), [128, 512], F32)) for i in range(4)]; Tpy = [T(f"mc_py{i}") for i in range(4)]
            wdst = [(wg[:, :, :].rearrange("p k n -> p (k n)"), Twg), (wu[:, :, :].rearrange("p k n -> p (k n)"), Twu),
                    (wd[:, :, :].rearrange("p k n -> p (k n)"), Twd)]

            def cast_w(e, j):
                q = (3 * e + j) % NSLOT
                dstap, Td = wdst[j]
                cx.op("act", lambda: nc.scalar.copy(out=dstap[:, 0:4096], in_=stg[q][:, 0:4096]), reads=[Tstg[q]], writes=[Td])
                cx.op("dve", lambda: nc.vector.tensor_copy(out=dstap[:, 4096:8192], in_=stg[q][:, 4096:8192]), reads=[Tstg[q]], writes=[Td])

            def gather_x(e):
                cx.gather(xe[e % 2][:, :], hs[:, :], tok[:, e:e + 1], reads=[Ths, Ttok], writes=[Txe[e % 2]], dst=Txe[e % 2])

            gather_x(0)
            npieces = 3 * NE

            def load_ahead(e, j):
                q = 3 * e + j + NSLOT
                if q < npieces:
                    load_w(q // 3, q % 3)

            for e in range(NE):
                b = e % 2
                if e + 1 < NE:
                    gather_x(e + 1)
                cast_w(e, 0)
                load_ahead(e, 0)
                xv = xe[b][:, :].rearrange("s (p k) -> s k p", k=NK)
                for half in range(2):
                    for c in range(8):
                        k = half * 8 + c
                        cx.op("pe", lambda: nc.tensor.transpose(out=ptr[half][:, c * 128:(c + 1) * 128], in_=xv[:, k, :], identity=g.identb[:, :]),
                              reads=[Txe[b], g.Tconst], writes=[Tptr[half]], signal=(c == 7))
                    if half == 0:
                        cx.op("act", lambda: nc.scalar.copy(out=xeT[:, 0:8, :], in_=ptr[0][:, :].rearrange("p (c t) -> p c t", c=8)), reads=[Tptr[0]], writes=[TxeT])
                    else:
                        cx.op("dve", lambda: nc.vector.tensor_copy(out=xeT[:, 8:16, :], in_=ptr[1][:, :].rearrange("p (c t) -> p c t", c=8)), reads=[Tptr[1]], writes=[TxeT])
                for k in range(NK):
                    cx.op("pe", lambda: nc.tensor.matmul(out=pg[:, :], lhsT=xeT[:, k, :], rhs=wg[:, k, :], start=(k == 0), stop=(k == NK - 1)),
                          reads=[TxeT, Twg], writes=[Tpg], signal=(k == NK - 1))
                cast_w(e, 1)
                load_ahead(e, 1)
                for k in range(NK):
                    cx.op("pe", lambda: nc.tensor.matmul(out=pu[:, :], lhsT=xeT[:, k, :], rhs=wu[:, k, :], start=(k == 0), stop=(k == NK - 1)),
                          reads=[TxeT, Twu], writes=[Tpu], signal=(k == NK - 1))
                cx.op("act", lambda: nc.scalar.activation(out=asb[:, :], in_=pg[:, :], func=AF.Silu), reads=[Tpg], writes=[Ta])
                cx.op("dve", lambda: nc.vector.tensor_tensor(out=abf[:, :], in0=pu[:, :], in1=asb[:, :], op=ALU.mult), reads=[Tpu, Ta], writes=[Tabf])
                av = abf[:, :].rearrange("s (p k) -> s k p", k=4)
                for c in range(4):
                    cx.op("pe", lambda: nc.tensor.transpose(out=ptr[0][:, c * 128:(c + 1) * 128], in_=av[:, c, :], identity=g.identb[:, :]),
                          reads=[Tabf, g.Tconst], writes=[Tptr[0]], signal=(c == 3))
                cx.op("act", lambda: nc.scalar.copy(out=aT[:, :, :], in_=ptr[0][:, 0:512].rearrange("p (c t) -> p c t", c=4)), reads=[Tptr[0]], writes=[TaT])
                cast_w(e, 2)
                load_ahead(e, 2)
                yb = ysb[b]; Tyb = Tysb[b]
                for nb in range(4):
                    for f in range(4):
                        cx.op("pe", lambda: nc.tensor.matmul(out=py[nb][:, :], lhsT=aT[:, f, :], rhs=wd[:, f, nb * 512:(nb + 1) * 512], start=(f == 0), stop=(f == 3)),
                              reads=[TaT, Twd], writes=[Tpy[nb]], signal=(f == 3))
                    if nb % 2 == 0:
                        cx.op("act", lambda: nc.scalar.copy(out=yb[:, nb * 512:(nb + 1) * 512], in_=py[nb][:, :]), reads=[Tpy[nb]], writes=[Tyb])
                    else:
                        cx.op("dve", lambda: nc.vector.tensor_copy(out=yb[:, nb * 512:(nb + 1) * 512], in_=py[nb][:, :]), reads=[Tpy[nb]], writes=[Tyb])
                cx.dma("sp", ys[e * CAP:(e + 1) * CAP, :], yb[:, :], reads=[Tyb], writes=[Tys], dst=Tys)
            cx.barrier()

        with ExitStack() as s2:
            sb2 = lambda n, s, d: s2.enter_context(nc.sbuf_tensor(U(n), s, d))
            xts = [sb2(f"md_xt{i}", [128, D], F32) for i in range(3)]; Txt = [T(f"md_xt{i}") for i in range(3)]
            ya = [sb2(f"md_ya{i}", [128, D], BF16) for i in range(2)]; Tya = [T(f"md_ya{i}") for i in range(2)]
            yb_ = [sb2(f"md_yb{i}", [128, D], BF16) for i in range(2)]; Tyb_ = [T(f"md_yb{i}") for i in range(2)]
            cx.dma("sp", xts[0][:, :], x_src[0:128, :], reads=[Tsrc], writes=[Txt[0]], dst=Txt[0])
            for i in range(NT):
                b = i % 2
                c3 = i % 3
                if i + 1 < NT:
                    n3 = (i + 1) % 3
                    cx.dma("sp", xts[n3][:, :], x_src[(i + 1) * 128:(i + 2) * 128, :], reads=[Tsrc], writes=[Txt[n3]], dst=Txt[n3])
                cx.gather(ya[b][:, :], ys[:, :], sidx[:, i, 0:1], reads=[Tys, Tsidx], writes=[Tya[b]], dst=Tya[b])
                cx.gather(yb_[b][:, :], ys[:, :], sidx[:, i, 1:2], reads=[Tys, Tsidx], writes=[Tyb_[b]], dst=Tyb_[b])
                cx.op("dve", lambda: nc.vector.scalar_tensor_tensor(out=xts[c3][:, :], in0=ya[b][:, :], scalar=cw[:, i, 0:1], in1=xts[c3][:, :], op0=ALU.mult, op1=ALU.add),
                      reads=[Tya[b], Tcw, Txt[c3]], writes=[Txt[c3]])
                cx.op("dve", lambda: nc.vector.scalar_tensor_tensor(out=xts[c3][:, :], in0=yb_[b][:, :], scalar=cw[:, i, 1:2], in1=xts[c3][:, :], op0=ALU.mult, op1=ALU.add),
                      reads=[Tyb_[b], Tcw, Txt[c3]], writes=[Txt[c3]])
                cx.dma("sp", x_dst[i * 128:(i + 1) * 128, :], xts[c3][:, :], reads=[Txt[c3]], writes=[Tdst], dst=Tdst)
            cx.barrier()


def build_program(phases=(1, 2, 3, 4, 5, 6), dbg=False):
    nc = bass.Bass("TRN2", target_bir_lowering=False)
    g = G()
    g.nc = nc
    dram = {}
    for name, shape in IN_SPECS.items():
        dram[name] = nc.dram_tensor(name, shape, F32, kind="ExternalInput").ap()
    for name, (shape, dt) in CONST_SPECS.items():
        dram[name] = nc.dram_tensor(name, shape, dt, kind="ExternalInput").ap()
    g.dram = dram
    kind = "ExternalOutput" if dbg else "Internal"
    y = nc.dram_tensor("y", [S, D], F32, kind="ExternalOutput").ap()
    x1 = nc.dram_tensor("x1", [S, D], F32, kind=kind).ap()
    x2 = nc.dram_tensor("x2", [S, D], F32, kind=kind).ap()
    x3 = nc.dram_tensor("x3", [S, D], F32, kind=kind).ap()
    mixT = nc.dram_tensor("mixT", [AW, S], BF16, kind=kind).ap()
    fT = nc.dram_tensor("fT", [D, S], BF16, kind=kind).ap()
    if dbg:
        g.dbg = {"tok": nc.dram_tensor("dbg_tok", [128, NE], I32, kind="ExternalOutput").ap(),
                 "sidx": nc.dram_tensor("dbg_sidx", [128, NT * 2], I32, kind="ExternalOutput").ap(),
                 "cum": nc.dram_tensor("dbg_cum", [128, NT * NE], F32, kind="ExternalOutput").ap(),
                 "cw": nc.dram_tensor("dbg_cw", [128, NT * 2], F32, kind="ExternalOutput").ap(),
                 "m12": nc.dram_tensor("dbg_m12", [128, NT * 2 * NE], F32, kind="ExternalOutput").ap()}
    g.hs = nc.dram_tensor("hs", [S + 1, D], BF16, kind="Internal").ap()
    g.ys = nc.dram_tensor("ys", [NE * CAP, D], BF16, kind="Internal").ap()
    g.Ths, g.Tys = T("hs"), T("ys")
    Tx0, Tx1, Tx2, Tx3, Ty, Tmix, TfT = T("x0"), T("x1"), T("x2"), T("x3"), T("y"), T("mixT"), T("fT")
    with ExitStack() as st:
        cx = Ctx(nc, st)
        g.cx = cx
        G.last_cx = cx
        sb = lambda n, s, d: st.enter_context(nc.sbuf_tensor(U(n), s, d))
        g.Tconst = T("consts")
        for name in ("identb", "identf", "rotP", "bmask", "tri", "iota_e", "iota_cb"):
            shape, dt = CONST_SPECS[name]
            t = sb("c_" + name, shape, dt)
            setattr(g, name, t)
            cx.dma("sp", t[:, :], dram[name][:, :], writes=[g.Tconst], dst=g.Tconst)
        xin = dram["x"]
        if 1 in phases:
            phase_attn(g, xin, mixT, Tmix)
        if 2 in phases:
            phase_outproj(g, mixT, Tmix, NH, dram["w_attn_out"], xin, Tx0, x1, Tx1)
        if 3 in phases:
            phase_moe(g, 0, x1, Tx1, x2, Tx2)
        if 4 in phases:
            phase_fourier(g, x2, fT, TfT)
        if 5 in phases:
            phase_outproj(g, fT, TfT, NK, dram["w_fourier_out"], x2, Tx2, x3, Tx3)
        if 6 in phases:
            phase_moe(g, 1, x3, Tx3, y, Ty)
        cx.barrier()
        cx.final_wait([t for t in (Ty, Tx1, Tx2, Tx3, Tmix, TfT) if t.dsem is not None])
    return nc


_CONSTS = None


def make_in_map(inputs, b, consts):
    m = {}
    for name, shape in IN_SPECS.items():
        a = np.asarray(inputs[name], dtype=np.float32)
        if name == "x":
            a = a[b]
        m[name] = np.ascontiguousarray(a.reshape(shape))
    m.update(consts)
    return m


def kernel(**inputs):
    global _CONSTS
    if _CONSTS is None:
        _CONSTS = make_consts()
    n = 8
    nc = build_program()
    in_maps = [make_in_map(inputs, b, _CONSTS) for b in range(n)]
    res = run_bass_kernel_spmd(nc, in_maps, core_ids=list(range(n)))
    out = np.stack([np.asarray(res.results[b]["y"], dtype=np.float32) for b in range(n)], axis=0)
    return out
```

```python
import math
from contextlib import ExitStack
import numpy as np
import ml_dtypes
import concourse.bass as bass
import concourse.mybir as mybir
from concourse.bass_utils import run_bass_kernel_spmd

F32 = mybir.dt.float32
BF16 = mybir.dt.bfloat16
I32 = mybir.dt.int32
U32 = mybir.dt.uint32
ALU = mybir.AluOpType
AF = mybir.ActivationFunctionType
AX = mybir.AxisListType

S = 2048
D = 2048
NT = S // 128
NK = D // 128
NH = 12
HD = 128
AW = NH * HD
NE = 64
DE = 512
CAP = 128
EPS = 1e-6
SHIFT = 6.0


_UID = [0]


def U(name):
    _UID[0] += 1
    return f"{name}_{_UID[0]}"


class T:
    def __init__(self, name, ap_fn=None):
        self.name = U(name)
        self.ap_fn = ap_fn
        self.w = {}
        self.r = {}
        self.dsem = None
        self.dcount = 0


class Ctx:
    def __init__(self, nc, stack):
        self.nc = nc
        self.stack = stack
        self.eng = {"pe": nc.tensor, "act": nc.scalar, "dve": nc.vector, "pool": nc.gpsimd, "sp": nc.sync}
        self.sem = {k: stack.enter_context(nc.semaphore("prog_" + k)) for k in self.eng}
        self.cnt = {k: 0 for k in self.eng}
        self.waited = {k: {} for k in self.eng}
        self.semobj = {("e", k): self.sem[k] for k in self.eng}
        self.dma_sems = []
        self.log = {k: [] for k in self.eng}

    def _wait(self, e, key, val):
        if val <= 0:
            return
        if key == ("e", e) and e == "pe":
            return
        if self.waited[e].get(key, 0) >= val:
            return
        if key[0] == "e":
            assert self.cnt[key[1]] >= val, f"wait on unsignaled {key} {val} > {self.cnt[key[1]]}"
        self.eng[e].wait_ge(self.semobj[key], val)
        self.waited[e][key] = val
        self.log[e].append(("w", key, val))

    def _deps(self, e, reads, writes):
        for t in reads:
            for k, v in t.w.items():
                self._wait(e, k, v)
        for t in writes:
            for k, v in t.w.items():
                self._wait(e, k, v)
            for k, v in t.r.items():
                if k == ("e", e):
                    continue
                self._wait(e, k, v)

    def op(self, e, fn, reads=(), writes=(), signal=True):
        self._deps(e, reads, writes)
        inst = fn()
        if signal:
            self.cnt[e] += 1
            inst.then_inc(self.sem[e], 1)
            seq = self.cnt[e]
            self.log[e].append(("s", ("e", e), 1))
        else:
            seq = self.cnt[e] + 1
        key = ("e", e)
        for t in reads:
            t.r[key] = max(t.r.get(key, 0), seq)
        for t in writes:
            t.r = {}
        for t in writes:
            t.w[key] = max(t.w.get(key, 0), seq)
        return inst

    def dma(self, q, out_ap, in_ap, reads=(), writes=(), dst=None, **kw):
        assert dst is not None
        if dst.dsem is None:
            dst.dsem = self.stack.enter_context(self.nc.semaphore("d_" + dst.name))
            self.semobj[("d", dst.name)] = dst.dsem
            self.dma_sems.append(dst)
        self._deps(q, reads, writes)
        inst = self.eng[q].dma_start(out=out_ap, in_=in_ap, **kw)
        dst.dcount += 16
        inst.then_inc(dst.dsem, 16)
        self.log[q].append(("s", ("d", dst.name), 16))
        key = ("d", dst.name)
        for t in reads:
            t.r[key] = max(t.r.get(key, 0), dst.dcount)
        for t in writes:
            t.r = {}
        for t in writes:
            t.w[key] = max(t.w.get(key, 0), dst.dcount)
        return inst

    def gather(self, out_ap, in_ap, idx_ap, reads=(), writes=(), dst=None):
        if dst.dsem is None:
            dst.dsem = self.stack.enter_context(self.nc.semaphore("d_" + dst.name))
            self.semobj[("d", dst.name)] = dst.dsem
            self.dma_sems.append(dst)
        self._deps("pool", reads, writes)
        inst = self.nc.gpsimd.indirect_dma_start(
            out=out_ap, out_offset=None, in_=in_ap,
            in_offset=bass.IndirectOffsetOnAxis(ap=idx_ap, axis=0))
        dst.dcount += 16
        inst.then_inc(dst.dsem, 16)
        self.log["pool"].append(("s", ("d", dst.name), 16))
        key = ("d", dst.name)
        for t in reads:
            t.r[key] = max(t.r.get(key, 0), dst.dcount)
        for t in writes:
            t.r = {}
        for t in writes:
            t.w[key] = max(t.w.get(key, 0), dst.dcount)
        return inst

    def barrier(self):
        for e in self.eng:
            for f in self.eng:
                if f != e:
                    self._wait(e, ("e", f), self.cnt[f])
            for t in self.dma_sems:
                self._wait(e, ("d", t.name), t.dcount)

    def final_wait(self, ts):
        for t in ts:
            self._wait("sp", ("d", t.name), t.dcount)


def make_consts():
    bf = ml_dtypes.bfloat16
    c = {}
    c["identb"] = np.eye(128, dtype=np.float32).astype(bf)
    c["identf"] = np.eye(128, dtype=np.float32)
    half = 16
    inv_freq = (500000.0 ** (-np.arange(0, 32, 2, dtype=np.float32) / 32)).astype(np.float32)
    ang = np.arange(S, dtype=np.float32)[:, None] * inv_freq[None, :]
    cosf = np.ones((128, S), np.float32)
    sinf = np.zeros((128, S), np.float32)
    cosf[0:16] = np.cos(ang).T
    cosf[16:32] = np.cos(ang).T
    sinf[0:16] = np.sin(ang).T
    sinf[16:32] = np.sin(ang).T
    c["ropec"] = cosf
    c["ropes"] = sinf
    rot = np.zeros((128, 128), np.float32)
    for m in range(16):
        rot[m + 16, m] = -1.0
        rot[m, m + 16] = 1.0
    c["rotP"] = rot.astype(bf)
    a = np.arange(128)[:, None]
    b = np.arange(128)[None, :]
    mL = (a - b >= 64)
    mD = (np.abs(a - b) <= 64)
    mU = (b - a >= 64)
    c["bmask"] = np.concatenate([mL, mD, mU], axis=1).astype(np.float32).astype(bf)
    ch = np.arange(256)
    angc = 2 * np.pi * ((ch[:, None] * ch[None, :]) % 256) / 256.0
    c["cs_c"] = np.concatenate([np.cos(angc) / 16.0, np.sin(angc) / 16.0], axis=1).astype(np.float32).astype(bf)
    t = np.arange(S, dtype=np.int64)
    angs = 2 * np.pi * ((t[:, None] * t[None, :]) % S) / float(S)
    sc = 1.0 / math.sqrt(S)
    c["dft_c"] = (np.cos(angs) * sc).astype(np.float32).astype(bf)
    c["dft_s"] = (-np.sin(angs) * sc).astype(np.float32).astype(bf)
    c["tri"] = (a <= b).astype(np.float32).astype(bf)
    c["iota_c"] = np.broadcast_to(np.arange(128, dtype=np.float32)[None, :], (128, 128)).copy()
    c["iota_e"] = np.broadcast_to((128.0 * np.arange(64, dtype=np.float32))[None, :], (128, 64)).copy()
    c["iota_cb"] = c["iota_c"].astype(bf)
    c["tokid"] = (128 * np.arange(NT, dtype=np.int32)[None, :] + np.arange(128, dtype=np.int32)[:, None]).astype(np.int32)
    c["fill2048"] = np.full((128, NE), S, np.int32)
    return c


CONST_SPECS = {
    "identb": ([128, 128], BF16), "identf": ([128, 128], F32), "ropec": ([128, S], F32), "ropes": ([128, S], F32),
    "rotP": ([128, 128], BF16), "bmask": ([128, 384], BF16), "cs_c": ([256, 512], BF16),
    "dft_c": ([S, S], BF16), "dft_s": ([S, S], BF16), "tri": ([128, 128], BF16),
    "iota_c": ([128, 128], F32), "iota_e": ([128, 64], F32), "iota_cb": ([128, 128], BF16),
    "tokid": ([128, NT], I32), "fill2048": ([128, NE], I32),
}

IN_SPECS = {
    "x": [S, D], "attn_norm_g": [1, D], "w_qkv": [D, 3 * AW], "q_norm_g": [1, HD], "k_norm_g": [1, HD],
    "w_attn_out": [AW, D], "fourier_norm_g": [1, D], "w_fourier_in": [D, D], "w_fourier_out": [D, D],
    "moe_norm_g": [2, D], "w_router_group": [2, D, 8], "b_router_group": [2, 8],
    "w_router_expert": [2, D, 64], "b_router_expert": [2, 64],
    "w_expert_gate": [2, NE, D, DE], "w_expert_up": [2, NE, D, DE], "w_expert_down": [2, NE, DE, D],
}


class G:
    pass


def phase_view(ap2d, d):
    return ap2d.rearrange("p (j r) -> p r j", r=d)


def blk512(ap2d, d, n):
    L = S // d
    v = phase_view(ap2d, d)
    if L >= 512:
        r = (512 * n) // L
        j0 = (512 * n) % L
        return v[:, r, j0:j0 + 512], False
    npb = 512 // L
    return v[:, n * npb:(n + 1) * npb, :], True


def blk128(ap2d, d, c):
    L = S // d
    v = phase_view(ap2d, d)
    r = (128 * c) // L
    j0 = (128 * c) % L
    return v[:, r, j0:j0 + 128]


def norm_to_hT(g, st, x_src, gvec_ap, hT, ThT):
    nc, cx = g.nc, g.cx
    with ExitStack() as s2:
        sb = lambda n, s, d: s2.enter_context(nc.sbuf_tensor(U(n), s, d))
        gb = sb("n_gb", [128, D], F32); Tgb = T("n_gb")
        junk = sb("n_junk", [128, D], BF16); Tjunk = T("n_junk")
        xts = [sb(f"n_xt{i}", [128, D], F32) for i in range(2)]; Txt = [T(f"n_xt{i}") for i in range(2)]
        hbs = [sb(f"n_hb{i}", [128, D], BF16) for i in range(2)]; Thb = [T(f"n_hb{i}") for i in range(2)]
        ss = sb("n_ss", [128, 2], F32); Tss = [T("n_ss0"), T("n_ss1")]
        rs = sb("n_rs", [128, 2], F32); Trs = [T("n_rs0"), T("n_rs1")]
        pts = [s2.enter_context(nc.psum_tensor(U(f"n_pt{i}"), [128, 1024], BF16)) for i in range(2)]
        Tpt = [T(f"n_pt{i}") for i in range(2)]
        cx.dma("sp", gb[:, :], gvec_ap.broadcast_to([128, D]), writes=[Tgb], dst=Tgb)

        def front(i):
            b = i % 2
            xt, hb = xts[b], hbs[b]
            cx.dma("sp", xt[:, :], x_src[i * 128:(i + 1) * 128, :], writes=[Txt[b]], dst=Txt[b])
            cx.op("act", lambda: nc.scalar.activation(out=junk[:, :], in_=xt[:, :], func=AF.Square, accum_out=ss[:, b:b + 1]),
                  reads=[Txt[b]], writes=[Tjunk, Tss[b]])
            cx.op("act", lambda: nc.scalar.activation(out=rs[:, b:b + 1], in_=ss[:, b:b + 1], func=AF.Sqrt, scale=1.0 / D, bias=EPS),
                  reads=[Tss[b]], writes=[Trs[b]])
            cx.op("dve", lambda: nc.vector.reciprocal(out=rs[:, b:b + 1], in_=rs[:, b:b + 1]), reads=[Trs[b]], writes=[Trs[b]])
            cx.op("dve", lambda: nc.vector.scalar_tensor_tensor(out=hb[:, :], in0=xt[:, :], scalar=rs[:, b:b + 1], in1=gb[:, :],
                                                                op0=ALU.mult, op1=ALU.mult),
                  reads=[Txt[b], Trs[b], Tgb], writes=[Thb[b]])

        def back(i):
            b = i % 2
            hb = hbs[b]
            for half in range(2):
                pt = pts[half]
                for c in range(8):
                    k = half * 8 + c
                    cx.op("pe", lambda: nc.tensor.transpose(out=pt[:, c * 128:(c + 1) * 128], in_=hb[:, k * 128:(k + 1) * 128],
                                                            identity=g.identb[:, :]),
                          reads=[Thb[b], g.Tconst], writes=[Tpt[half]], signal=(c == 7))
                if half == 0:
                    cx.op("act", lambda: nc.scalar.copy(out=hT[:, half * 8:(half + 1) * 8, i * 128:(i + 1) * 128],
                                                        in_=pt[:, :].rearrange("p (c t) -> p c t", c=8)),
                          reads=[Tpt[half]], writes=[ThT])
                else:
                    cx.op("dve", lambda: nc.vector.tensor_copy(out=hT[:, half * 8:(half + 1) * 8, i * 128:(i + 1) * 128],
                                                               in_=pt[:, :].rearrange("p (c t) -> p c t", c=8)),
                          reads=[Tpt[half]], writes=[ThT])

        front(0)
        for i in range(NT):
            if i + 1 < NT:
                front(i + 1)
            back(i)
        cx.barrier()


def cast_piece(g, idx, out_ap, in_ap, Tin, Tout):
    nc, cx = g.nc, g.cx
    e = ("act", "dve")[idx % 2]
    if e == "act":
        cx.op("act", lambda: nc.scalar.copy(out=out_ap, in_=in_ap), reads=[Tin], writes=[Tout])
    else:
        cx.op("dve", lambda: nc.vector.tensor_copy(out=out_ap, in_=in_ap), reads=[Tin], writes=[Tout])


def phase_attn(g, x_src, mixT_dram, Tmix):
    nc, cx = g.nc, g.cx
    dram = g.dram
    DIL = (1, 4, 16)
    with ExitStack() as st:
        sb = lambda n, s, d: st.enter_context(nc.sbuf_tensor(U(n), s, d))
        hT = sb("a_hT", [128, NK, S], BF16); ThT = T("a_hT")
        norm_to_hT(g, st, x_src, dram["attn_norm_g"][0:1, :], hT, ThT)
        ropec = sb("a_ropec", [128, S], F32); ropes = sb("a_ropes", [128, S], F32); Trope = T("a_rope")
        qgc = sb("a_qg", [128, 2], F32); Tqg = T("a_qg")
        wst = [sb(f"a_wst{j}", [128, NK, 128], F32) for j in range(3)]; Twst = [T(f"a_wst{j}") for j in range(3)]
        wbf = [[sb(f"a_wbf{b}_{j}", [128, NK, 128], BF16) for j in range(3)] for b in range(2)]
        Twbf = [[T(f"a_wbf{b}_{j}") for j in range(3)] for b in range(2)]
        qk = [sb(f"a_qk{j}", [128, S], BF16) for j in range(2)]; Tqk = [T(f"a_qk{j}") for j in range(2)]
        vsb = sb("a_v", [128, NT, 128], BF16); Tv = T("a_v")
        sq = [sb(f"a_sq{i}", [128, 512], F32) for i in range(2)]; Tsq = [T(f"a_sq{i}") for i in range(2)]
        rq = [sb(f"a_rq{i}", [128, 512], F32) for i in range(2)]; Trq = [T(f"a_rq{i}") for i in range(2)]
        qgb = [sb(f"a_qgb{i}", [128, 512], BF16) for i in range(2)]; Tqgb = [T(f"a_qgb{i}") for i in range(2)]
        t1 = [sb(f"a_t1{i}", [128, 512], F32) for i in range(2)]; Tt1 = [T(f"a_t1{i}") for i in range(2)]
        t2 = [sb(f"a_t2{i}", [128, 512], F32) for i in range(2)]; Tt2 = [T(f"a_t2{i}") for i in range(2)]
        pTs = [sb(f"a_pT{j}", [128, 384], BF16) for j in range(2)]; TpT = [T(f"a_pT{j}") for j in range(2)]
        onat = sb("a_onat", [128, 3, S], BF16); Tonat = [T(f"a_onat{j}") for j in range(3)]
        Ltot = sb("a_L", [128, S], F32); TL = T("a_L")
        mixo = sb("a_mixo", [128, 3, S], BF16); Tmixo = [T(f"a_mixo{j}") for j in range(3)]
        onesf = sb("a_onesf", [128, 128], F32); onesb = sb("a_onesb", [128, 128], BF16); Tones = T("a_ones")
        bank = [st.enter_context(nc.psum_tensor(U(f"a_ps{j}"), [128, 512], F32)) for j in range(8)]
        Tb = [T(f"a_ps{j}") for j in range(8)]
        PQ0, PQ1, PSS, PROT, PV, PST, PO, PL = range(8)

        cx.dma("sp", ropec[:, :], dram["ropec"][:, :], writes=[Trope], dst=Trope)
        cx.dma("sp", ropes[:, :], dram["ropes"][:, :], writes=[Trope], dst=Trope)
        with nc.allow_non_contiguous_dma(reason="tiny gain columns"):
            cx.dma("sp", qgc[:, 0:1], dram["q_norm_g"].rearrange("o d -> d o"), writes=[Tqg], dst=Tqg)
            cx.dma("sp", qgc[:, 1:2], dram["k_norm_g"].rearrange("o d -> d o"), writes=[Tqg], dst=Tqg)
        cx.op("dve", lambda: nc.vector.memset(onesf[:, :], 1.0), writes=[Tones])
        cx.op("dve", lambda: nc.vector.memset(onesb[:, :], 1.0), writes=[Tones])

        wq = dram["w_qkv"]
        heads = [(hg, gi) for hg in range(4) for gi in range(3)]

        def load_w(hidx):
            hg, gi = heads[hidx]
            h = gi * 4 + hg
            for j in range(3):
                cx.dma("sp", wst[j][:, :, :], wq[:, j * AW + h * 128: j * AW + (h + 1) * 128].rearrange("(k p) n -> p k n", p=128),
                       writes=[Twst[j]], dst=Twst[j])

        def cast_w(hidx):
            b = hidx % 2
            for j in range(3):
                cast_piece(g, j, wbf[b][j][:, :, :], wst[j][:, :, :], Twst[j], Twbf[b][j])

        load_w(0)
        cast_w(0)
        for hidx, (hg, gi) in enumerate(heads):
            d = DIL[gi]
            L = S // d
            bps = L // 128
            wb = wbf[hidx % 2]
            Twb = Twbf[hidx % 2]
            if hidx + 1 < len(heads):
                load_w(hidx + 1)
            blocks = [(j, n) for j in range(2) for n in range(4)]

            def qk_front(bi):
                j, n = blocks[bi]
                p = bi % 2
                pq = bank[PQ0 + p]; Tpq = Tb[PQ0 + p]
                for k in range(NK):
                    cx.op("pe", lambda: nc.tensor.matmul(out=pq[:, :], lhsT=wb[j][:, k, :], rhs=hT[:, k, n * 512:(n + 1) * 512], start=(k == 0), stop=(k == NK - 1)),
                          reads=[ThT, Twb[j]], writes=[Tpq], signal=(k == NK - 1))
                cx.op("act", lambda: nc.scalar.activation(out=sq[p][:, :], in_=pq[:, :], func=AF.Square), reads=[Tpq], writes=[Tsq[p]])
                cx.op("act", lambda: nc.scalar.mul(out=qgb[p][:, :], in_=pq[:, :], mul=qgc[:, j:j + 1]), reads=[Tpq, Tqg], writes=[Tqgb[p]])

            def qk_back(bi):
                j, n = blocks[bi]
                p = bi % 2
                pss = bank[(PSS, PST)[p]]; Tpss = Tb[(PSS, PST)[p]]
                prot = bank[(PROT, PV)[p]]; Tprot = Tb[(PROT, PV)[p]]
                cx.op("pe", lambda: nc.tensor.matmul(out=pss[:, :], lhsT=onesf[:, :], rhs=sq[p][:, :], start=True, stop=True),
                      reads=[Tsq[p], Tones], writes=[Tpss])
                cx.op("pe", lambda: nc.tensor.matmul(out=prot[:, :], lhsT=g.rotP[:, :], rhs=qgb[p][:, :], start=True, stop=True),
                      reads=[Tqgb[p], g.Tconst], writes=[Tprot])
                cx.op("act", lambda: nc.scalar.activation(out=rq[p][:, :], in_=pss[:, :], func=AF.Ln, scale=1.0 / HD, bias=EPS),
                      reads=[Tpss], writes=[Trq[p]])
                cx.op("act", lambda: nc.scalar.activation(out=rq[p][:, :], in_=rq[p][:, :], func=AF.Exp, scale=-0.5), reads=[Trq[p]], writes=[Trq[p]])
                cx.op("dve", lambda: nc.vector.tensor_tensor(out=t1[p][:, :], in0=qgb[p][:, :], in1=ropec[:, n * 512:(n + 1) * 512], op=ALU.mult),
                      reads=[Tqgb[p], Trope], writes=[Tt1[p]])
                cx.op("dve", lambda: nc.vector.tensor_tensor(out=t2[p][:, :], in0=prot[:, :], in1=ropes[:, n * 512:(n + 1) * 512], op=ALU.mult),
                      reads=[Tprot, Trope], writes=[Tt2[p]])
                cx.op("pool", lambda: nc.gpsimd.tensor_tensor(out=t1[p][:, :], in0=t1[p][:, :], in1=t2[p][:, :], op=ALU.add),
                      reads=[Tt1[p], Tt2[p]], writes=[Tt1[p]])
                if d == 1:
                    oap = qk[j][:, n * 512:(n + 1) * 512]
                    i0 = t1[p][:, :]; i1 = rq[p][:, :]
                else:
                    w_ = 512 // d
                    oap = qk[j][:, :].rearrange("p (r j) -> p r j", r=d)[:, :, n * w_:(n + 1) * w_]
                    i0 = t1[p][:, :].rearrange("p (jj r) -> p r jj", r=d)
                    i1 = rq[p][:, :].rearrange("p (jj r) -> p r jj", r=d)
                cx.op("dve", lambda: nc.vector.tensor_tensor(out=oap, in0=i0, in1=i1, op=ALU.mult),
                      reads=[Tt1[p], Trq[p]], writes=[Tqk[j]])

            for bi in range(len(blocks) + 1):
                if bi < len(blocks):
                    qk_front(bi)
                if bi >= 1:
                    qk_back(bi - 1)
            for c4 in range(4):
                for cc in range(4):
                    c = c4 * 4 + cc
                    for k in range(NK):
                        cx.op("pe", lambda: nc.tensor.matmul(out=bank[PV][:, cc * 128:(cc + 1) * 128], lhsT=blk128(hT[:, k, :], d, c),
                                                             rhs=wb[2][:, k, :], start=(k == 0), stop=(k == NK - 1)),
                              reads=[ThT, Twb[2]], writes=[Tb[PV]], signal=(k == NK - 1 and cc == 3))
                cx.op("act", lambda: nc.scalar.copy(out=vsb[:, c4 * 4:(c4 + 1) * 4, :], in_=bank[PV][:, :].rearrange("p (a b) -> p a b", b=128)),
                      reads=[Tb[PV]], writes=[Tv])
            if hidx + 1 < len(heads):
                cast_w(hidx + 1)
            r3 = (lambda ap: ap.rearrange("p (a b) -> p a b", b=L)) if L < 512 else (lambda ap: ap)

            def keychunks(i):
                return [c for c in (i - 1, i, i + 1) if 0 <= c < NT and c // bps == i // bps]

            def att_front(i):
                cs = keychunks(i)
                lo = cs[0] - (i - 1)
                nc_ = len(cs)
                pT = pTs[i % 2]; TpTi = TpT[i % 2]
                pst = bank[(PST, PSS)[i % 2]]; Tpst = Tb[(PST, PSS)[i % 2]]
                for jj, c in enumerate(cs):
                    cx.op("pe", lambda: nc.tensor.matmul(out=pst[:, jj * 128:(jj + 1) * 128], lhsT=qk[1][:, c * 128:(c + 1) * 128],
                                                         rhs=qk[0][:, i * 128:(i + 1) * 128], start=True, stop=True),
                          reads=[Tqk[0], Tqk[1]], writes=[Tpst], signal=(jj == nc_ - 1))
                cx.op("act", lambda: nc.scalar.activation(out=pT[:, 0:nc_ * 128], in_=pst[:, 0:nc_ * 128], func=AF.Exp,
                                                          scale=float(HD) ** -0.5, bias=-SHIFT),
                      reads=[Tpst], writes=[TpTi])
                cx.op("dve", lambda: nc.vector.tensor_tensor(out=pT[:, 0:nc_ * 128], in0=pT[:, 0:nc_ * 128],
                                                             in1=g.bmask[:, lo * 128:(lo + nc_) * 128], op=ALU.mult),
                      reads=[TpTi, g.Tconst], writes=[TpTi])

            def att_back(i):
                cs = keychunks(i)
                nc_ = len(cs)
                n, ii = i // 4, i % 4
                pT = pTs[i % 2]; TpTi = TpT[i % 2]
                po = bank[(PO, PROT)[n % 2]]; Tpo = Tb[(PO, PROT)[n % 2]]
                pl = bank[(PL, PV)[n % 2]]; Tpl = Tb[(PL, PV)[n % 2]]
                for jj, c in enumerate(cs):
                    cx.op("pe", lambda: nc.tensor.matmul(out=po[:, ii * 128:(ii + 1) * 128], lhsT=vsb[:, c, :], rhs=pT[:, jj * 128:(jj + 1) * 128],
                                                         start=(jj == 0), stop=(jj == nc_ - 1)),
                          reads=[Tv, TpTi], writes=[Tpo], signal=False)
                for jj, c in enumerate(cs):
                    cx.op("pe", lambda: nc.tensor.matmul(out=pl[:, ii * 128:(ii + 1) * 128], lhsT=onesb[:, :], rhs=pT[:, jj * 128:(jj + 1) * 128],
                                                         start=(jj == 0), stop=(jj == nc_ - 1)),
                          reads=[Tones, TpTi], writes=[Tpl], signal=(jj == nc_ - 1))
                if ii == 3:
                    oview, _ = blk512(onat[:, gi, :], d, n)
                    lview, _ = blk512(Ltot[:, :], d, n)
                    cx.op("act", lambda: nc.scalar.copy(out=oview, in_=r3(po[:, :])), reads=[Tpo], writes=[Tonat[gi]])
                    if gi == 0:
                        cx.op("dve", lambda: nc.vector.tensor_copy(out=lview, in_=r3(pl[:, :])), reads=[Tpl], writes=[TL])
                    else:
                        cx.op("dve", lambda: nc.vector.tensor_tensor(out=lview, in0=r3(pl[:, :]), in1=lview, op=ALU.add),
                              reads=[Tpl, TL], writes=[TL])

            for i in range(NT + 1):
                if i < NT:
                    att_front(i)
                if i >= 1:
                    att_back(i - 1)
            if gi == 2:
                cx.op("act", lambda: nc.scalar.activation(out=Ltot[:, :], in_=Ltot[:, :], func=AF.Ln), reads=[TL], writes=[TL])
                cx.op("act", lambda: nc.scalar.activation(out=Ltot[:, :], in_=Ltot[:, :], func=AF.Exp, scale=-1.0), reads=[TL], writes=[TL])
                for g2 in range(3):
                    e = ("dve", "pool", "dve")[g2]
                    eng = nc.vector if e == "dve" else nc.gpsimd
                    cx.op(e, lambda: eng.tensor_tensor(out=mixo[:, g2, :], in0=onat[:, g2, :], in1=Ltot[:, :], op=ALU.mult),
                          reads=[Tonat[g2], TL], writes=[Tmixo[g2]])
                    h = g2 * 4 + hg
                    cx.dma("sp", mixT_dram[h * 128:(h + 1) * 128, :], mixo[:, g2, :], reads=[Tmixo[g2]], writes=[Tmix], dst=Tmix)
        cx.barrier()


def phase_outproj(g, srcT_dram, Tsrc, KC, w_dram, x_old, Told, x_new, Tnew):
    nc, cx = g.nc, g.cx
    with ExitStack() as st:
        sb = lambda n, s, d: st.enter_context(nc.sbuf_tensor(U(n), s, d))
        sT = sb("o_sT", [128, KC, S], BF16); TsT = T("o_sT")
        wst = [sb(f"o_wst{j}", [128, 4, 512], F32) for j in range(2)]; Twst = [T(f"o_wst{j}") for j in range(2)]
        wbf = [sb(f"o_wbf{j}", [128, KC, 512], BF16) for j in range(2)]; Twbf = [T(f"o_wbf{j}") for j in range(2)]
        xts = [sb(f"o_xt{j}", [128, 512], F32) for j in range(4)]; Txt = [T(f"o_xt{j}") for j in range(4)]
        bank = [st.enter_context(nc.psum_tensor(U(f"o_ps{j}"), [128, 512], F32)) for j in range(4)]
        Tb = [T(f"o_ps{j}") for j in range(4)]
        for k in range(KC):
            cx.dma("sp", sT[:, k, :], srcT_dram[k * 128:(k + 1) * 128, :], reads=[Tsrc], writes=[TsT], dst=TsT)
        wv = w_dram.rearrange("(k p) n -> p k n", p=128)
        npieces = KC // 4
        pc = 0

        def load_cast(nb):
            nonlocal pc
            for q in range(npieces):
                j = pc % 2
                pc += 1
                cx.dma("sp", wst[j][:, :, :], wv[:, q * 4:(q + 1) * 4, nb * 512:(nb + 1) * 512], writes=[Twst[j]], dst=Twst[j])
                cast_piece(g, pc, wbf[nb % 2][:, q * 4:(q + 1) * 4, :], wst[j][:, :, :], Twst[j], Twbf[nb % 2])

        load_cast(0)
        seq = [(nb, i) for nb in range(4) for i in range(NT)]

        def load_x(q):
            nb_, i_ = seq[q]
            cx.dma("sp", xts[q % 4][:, :], x_old[i_ * 128:(i_ + 1) * 128, nb_ * 512:(nb_ + 1) * 512], reads=[Told], writes=[Txt[q % 4]], dst=Txt[q % 4])

        load_x(0)
        load_x(1)
        it = 0
        for nb in range(4):
            if nb + 1 < 4:
                load_cast(nb + 1)
            for i in range(NT):
                pb = bank[it % 4]; Tpb = Tb[it % 4]
                xt = xts[it % 4]; Txi = Txt[it % 4]
                if it + 2 < len(seq):
                    load_x(it + 2)
                it += 1
                for k in range(KC):
                    cx.op("pe", lambda: nc.tensor.matmul(out=pb[:, :], lhsT=sT[:, k, i * 128:(i + 1) * 128], rhs=wbf[nb % 2][:, k, :],
                                                         start=(k == 0), stop=(k == KC - 1)),
                          reads=[TsT, Twbf[nb % 2]], writes=[Tpb], signal=(k == KC - 1))
                cx.op("dve", lambda: nc.vector.tensor_tensor(out=xt[:, :], in0=pb[:, :], in1=xt[:, :], op=ALU.add),
                      reads=[Tpb, Txi], writes=[Txi])
                cx.dma("sp", x_new[i * 128:(i + 1) * 128, nb * 512:(nb + 1) * 512], xt[:, :], reads=[Txi], writes=[Tnew], dst=Tnew)
        cx.barrier()


def phase_fourier(g, x_src, fT_dram, TfT):
    nc, cx = g.nc, g.cx
    dram = g.dram
    with ExitStack() as st:
        sb = lambda n, s, d: st.enter_context(nc.sbuf_tensor(U(n), s, d))
        hT = sb("f_hT", [128, NK, S], BF16); ThT = T("f_hT")
        norm_to_hT(g, st, x_src, dram["fourier_norm_g"][0:1, :], hT, ThT)
        wst = sb("f_wst", [128, NK, 256], F32); Twst = T("f_wst")
        wbf = [sb(f"f_wbf{j}", [128, NK, 256], BF16) for j in range(2)]; Twbf = [T(f"f_wbf{j}") for j in range(2)]
        csc = sb("f_csc", [128, 2, 512], BF16); Tcsc = T("f_csc")
        uT = sb("f_uT", [128, 2, S], BF16); TuT = T("f_uT")
        vsb = sb("f_v", [128, NT, 512], BF16); Tv = T("f_v")
        tabs = [[sb(f"f_tab{b}_{j}", [128, NT, 512], BF16) for j in range(2)] for b in range(2)]
        Ttab = [[T(f"f_tab{b}_{j}") for j in range(2)] for b in range(2)]
        fo = [sb(f"f_fo{j}", [128, 512], BF16) for j in range(2)]; Tfo = [T(f"f_fo{j}") for j in range(2)]
        bank = [st.enter_context(nc.psum_tensor(U(f"f_ps{j}"), [128, 512], F32)) for j in range(4)]
        Tb = [T(f"f_ps{j}") for j in range(4)]
        cx.dma("sp", csc[:, :, :], dram["cs_c"].rearrange("(m p) n -> p m n", p=128), writes=[Tcsc], dst=Tcsc)
        wv = dram["w_fourier_in"].rearrange("(k p) n -> p k n", p=128)
        dftv = [dram["dft_c"].rearrange("(i p) s -> p i s", p=128), dram["dft_s"].rearrange("(i p) s -> p i s", p=128)]

        def load_w(gi):
            cx.dma("sp", wst[:, :, :], wv[:, :, gi * 256:(gi + 1) * 256], writes=[Twst], dst=Twst)
            cx.op("act", lambda: nc.scalar.copy(out=wbf[gi % 2][:, 0:8, :], in_=wst[:, 0:8, :]), reads=[Twst], writes=[Twbf[gi % 2]])
            cx.op("dve", lambda: nc.vector.tensor_copy(out=wbf[gi % 2][:, 8:16, :], in_=wst[:, 8:16, :]), reads=[Twst], writes=[Twbf[gi % 2]])

        tcount = 0

        def load_tab(n):
            nonlocal tcount
            b = tcount % 2
            tcount += 1
            for j in range(2):
                cx.dma("sp", tabs[b][j][:, :, :], dftv[j][:, :, n * 512:(n + 1) * 512], writes=[Ttab[b][j]], dst=Ttab[b][j])
            return b

        load_w(0)
        it = 0
        for gi in range(8):
            if gi + 1 < 8:
                load_w(gi + 1)
            for m in range(2):
                for n in range(4):
                    pb = bank[it % 4]; Tpb = Tb[it % 4]; it += 1
                    for k in range(NK):
                        cx.op("pe", lambda: nc.tensor.matmul(out=pb[:, :], lhsT=wbf[gi % 2][:, k, m * 128:(m + 1) * 128], rhs=hT[:, k, n * 512:(n + 1) * 512],
                                                             start=(k == 0), stop=(k == NK - 1)),
                              reads=[ThT, Twbf[gi % 2]], writes=[Tpb], signal=(k == NK - 1))
                    e = ("act", "dve")[it % 2]
                    if e == "act":
                        cx.op("act", lambda: nc.scalar.copy(out=uT[:, m, n * 512:(n + 1) * 512], in_=pb[:, :]), reads=[Tpb], writes=[TuT])
                    else:
                        cx.op("dve", lambda: nc.vector.tensor_copy(out=uT[:, m, n * 512:(n + 1) * 512], in_=pb[:, :]), reads=[Tpb], writes=[TuT])
            for i in range(NT):
                pb = bank[it % 4]; Tpb = Tb[it % 4]; it += 1
                for m in range(2):
                    cx.op("pe", lambda: nc.tensor.matmul(out=pb[:, :], lhsT=uT[:, m, i * 128:(i + 1) * 128], rhs=csc[:, m, :], start=(m == 0), stop=(m == 1)),
                          reads=[TuT, Tcsc], writes=[Tpb], signal=(m == 1))
                e = ("act", "dve")[it % 2]
                if e == "act":
                    cx.op("act", lambda: nc.scalar.copy(out=vsb[:, i, :], in_=pb[:, :]), reads=[Tpb], writes=[Tv])
                else:
                    cx.op("dve", lambda: nc.vector.tensor_copy(out=vsb[:, i, :], in_=pb[:, :]), reads=[Tpb], writes=[Tv])
            for n in range(4):
                if gi == 0 and n == 0:
                    nxt_b = load_tab(0)
                b = nxt_b
                if not (gi == 7 and n == 3):
                    nxt_b = load_tab((n + 1) % 4)
                for m in range(2):
                    pb = bank[it % 4]; Tpb = Tb[it % 4]; it += 1
                    for i in range(NT):
                        for j in range(2):
                            cx.op("pe", lambda: nc.tensor.matmul(out=pb[:, :], lhsT=vsb[:, i, j * 256 + m * 128: j * 256 + (m + 1) * 128],
                                                                 rhs=tabs[b][j][:, i, :], start=(i == 0 and j == 0), stop=(i == NT - 1 and j == 1)),
                                  reads=[Tv, Ttab[b][j]], writes=[Tpb], signal=(i == NT - 1 and j == 1))
                    f = fo[it % 2]; Tf = Tfo[it % 2]
                    e = ("act", "dve")[it % 2]
                    if e == "act":
                        cx.op("act", lambda: nc.scalar.copy(out=f[:, :], in_=pb[:, :]), reads=[Tpb], writes=[Tf])
                    else:
                        cx.op("dve", lambda: nc.vector.tensor_copy(out=f[:, :], in_=pb[:, :]), reads=[Tpb], writes=[Tf])
                    row = gi * 256 + m * 128
                    cx.dma("sp", fT_dram[row:row + 128, n * 512:(n + 1) * 512], f[:, :], reads=[Tf], writes=[TfT], dst=TfT)
        cx.barrier()


def phase_moe(g, layer, x_src, Tsrc, x_dst, Tdst):
    nc, cx = g.nc, g.cx
    dram = g.dram
    hs, Ths, ys, Tys = g.hs, g.Ths, g.ys, g.Tys
    BIG = 1.0e30
    with ExitStack() as st:
        sb = lambda n, s, d: st.enter_context(nc.sbuf_tensor(U(n), s, d))
        maskall = sb("m_mask", [128, NT, NE], BF16); Tmask = T("m_mask")
        cw = sb("m_cw", [128, NT, 2], F32); Tcw = T("m_cw")
        sidf = sb("m_sidf", [128, NT, 2], F32); Tsidf = T("m_sidf")
        sidx = sb("m_sidx", [128, NT, 2], I32); Tsidx = T("m_sidx")
        tok = sb("m_tok", [128, NE], I32); Ttok = T("m_tok")
        onesb = sb("m_onesb", [128, 128], BF16); Tones = T("m_ones")
        cx.op("dve", lambda: nc.vector.memset(onesb[:, :], 1.0), writes=[Tones])
        NSLOT = 4
        stg = [sb(f"mc_stg{i}", [128, 8192], F32) for i in range(NSLOT)]; Tstg = [T(f"mc_stg{i}") for i in range(NSLOT)]
        weg = dram["w_expert_gate"]; weu = dram["w_expert_up"]; wed = dram["w_expert_down"]
        wsrc = [lambda e: weg[layer, e].rearrange("(p k) n -> p (k n)", p=128),
                lambda e: weu[layer, e].rearrange("(p k) n -> p (k n)", p=128),
                lambda e: wed[layer, e].rearrange("(p k) n -> p (k n)", p=128)]

        def load_w(e, j):
            q = (3 * e + j) % NSLOT
            cx.dma("sp", stg[q][:, :], wsrc[j](e), writes=[Tstg[q]], dst=Tstg[q])

        for q0 in range(NSLOT):
            load_w(q0 // 3, q0 % 3)
        sAB = ExitStack()
        m12 = sAB.enter_context(nc.sbuf_tensor(U("m_m12"), [128, NT, 2, NE], F32)); Tm12 = T("m_m12")
        cum = sAB.enter_context(nc.sbuf_tensor(U("m_cum"), [128, NT, NE], F32)); Tcum = T("m_cum")

        with ExitStack() as s2:
            sb2 = lambda n, s, d: s2.enter_context(nc.sbuf_tensor(U(n), s, d))
            gb = sb2("ma_gb", [128, D], F32); Tgb = T("ma_gb")
            gcol = sb2("ma_gcol", [128, NK], F32); Tgcol = T("ma_gcol")
            wr = sb2("ma_wr", [128, NK, 72], F32); Twr = T("ma_wr")
            bias = sb2("ma_bias", [128, 72], F32); Tbias = T("ma_bias")
            zrow = sb2("ma_zrow", [1, D], BF16); Tz = T("ma_zrow")
            xts = [sb2(f"ma_xt{i}", [128, D], F32) for i in range(2)]; Txt = [T(f"ma_xt{i}") for i in range(2)]
            hbs = [sb2(f"ma_hb{i}", [128, D], BF16) for i in range(2)]; Thb = [T(f"ma_hb{i}") for i in range(2)]
            xT = sb2("ma_xT", [128, NK, 128], F32); TxT = T("ma_xT")
            sms = [sb2(f"ma_sm{i}", [128, 4], F32) for i in range(2)]; Tsms = [T(f"ma_sm{i}") for i in range(2)]
            lgA = sb2("ma_lgA", [128, NT, 72], F32); TlgA = T("ma_lgA")
            r16 = sb2("ma_r16", [128, 8, NT], F32); Tr16 = T("ma_r16")
            gm = sb2("ma_gm", [128, NT, 8], F32); Tgm = T("ma_gm")
            ce = sb2("ma_ce", [128, NT, 8], F32); Tce = T("ma_ce")
            fm = sb2("ma_fm", [128, NT, NE], F32); Tfm = T("ma_fm")
            fm2 = fm; Tfm2 = Tfm
            pxt = [s2.enter_context(nc.psum_tensor(U(f"ma_px{i}"), [128, 512], F32)) for i in range(4)]
            Tpx = [T(f"ma_px{i}") for i in range(4)]
            plg = s2.enter_context(nc.psum_tensor(U("ma_plg"), [128, 72], F32)); Tplg = T("ma_plg")

            cx.dma("sp", gb[:, :], dram["moe_norm_g"][layer:layer + 1, :].broadcast_to([128, D]), writes=[Tgb], dst=Tgb)
            with nc.allow_non_contiguous_dma(reason="small router tables"):
                cx.dma("sp", gcol[:, :], dram["moe_norm_g"][layer].rearrange("(k p) -> p k", p=128), writes=[Tgcol], dst=Tgcol)
                cx.dma("sp", wr[:, :, 0:8], dram["w_router_group"][layer].rearrange("(k p) n -> p k n", p=128), writes=[Twr], dst=Twr)
                cx.dma("sp", wr[:, :, 8:72], dram["w_router_expert"][layer].rearrange("(k p) n -> p k n", p=128), writes=[Twr], dst=Twr)
                cx.dma("sp", bias[:, 0:8], dram["b_router_group"][layer:layer + 1, :].broadcast_to([128, 8]), writes=[Tbias], dst=Tbias)
                cx.dma("sp", bias[:, 8:72], dram["b_router_expert"][layer:layer + 1, :].broadcast_to([128, 64]), writes=[Tbias], dst=Tbias)
            cx.op("dve", lambda: nc.vector.memset(zrow[:, :], 0.0), writes=[Tz])
            cx.dma("sp", hs[S:S + 1, :], zrow[:, :], reads=[Tz], writes=[Ths], dst=Ths)
            for k in range(NK):
                cx.op("dve", lambda: nc.vector.tensor_scalar(out=wr[:, k, :], in0=wr[:, k, :], scalar1=gcol[:, k:k + 1], scalar2=None, op0=ALU.mult),
                      reads=[Twr, Tgcol], writes=[Twr])
            def a_front(i):
                b = i % 2
                xt, hb = xts[b], hbs[b]
                sm, Tsm = sms[b], Tsms[b]
                cx.dma("sp", xt[:, :], x_src[i * 128:(i + 1) * 128, :], reads=[Tsrc], writes=[Txt[b]], dst=Txt[b])
                cx.op("act", lambda: nc.scalar.activation(out=hb[:, :], in_=xt[:, :], func=AF.Square, accum_out=sm[:, 0:1]),
                      reads=[Txt[b]], writes=[Thb[b], Tsm])
                cx.op("act", lambda: nc.scalar.activation(out=sm[:, 1:2], in_=sm[:, 0:1], func=AF.Sqrt, scale=1.0 / D, bias=EPS),
                      reads=[Tsm], writes=[Tsm])
                cx.op("dve", lambda: nc.vector.reciprocal(out=sm[:, 1:2], in_=sm[:, 1:2]), reads=[Tsm], writes=[Tsm])
                cx.op("dve", lambda: nc.vector.scalar_tensor_tensor(out=hb[:, :], in0=xt[:, :], scalar=sm[:, 1:2], in1=gb[:, :],
                                                                    op0=ALU.mult, op1=ALU.mult),
                      reads=[Txt[b], Tsm, Tgb], writes=[Thb[b]])
                cx.dma("sp", hs[i * 128:(i + 1) * 128, :], hb[:, :], reads=[Thb[b]], writes=[Ths], dst=Ths)

            def a_back(i):
                b = i % 2
                xt = xts[b]
                sm, Tsm = sms[b], Tsms[b]
                for q in range(4):
                    for c in range(4):
                        k = q * 4 + c
                        cx.op("pe", lambda: nc.tensor.transpose(out=pxt[q][:, c * 128:(c + 1) * 128], in_=xt[:, k * 128:(k + 1) * 128], identity=g.identf[:, :]),
                              reads=[Txt[b], g.Tconst], writes=[Tpx[q]], signal=(c == 3))
                    if q % 2 == 0:
                        cx.op("act", lambda: nc.scalar.copy(out=xT[:, q * 4:(q + 1) * 4, :], in_=pxt[q][:, :].rearrange("p (c t) -> p c t", c=4)),
                              reads=[Tpx[q]], writes=[TxT])
                    else:
                        cx.op("dve", lambda: nc.vector.tensor_copy(out=xT[:, q * 4:(q + 1) * 4, :], in_=pxt[q][:, :].rearrange("p (c t) -> p c t", c=4)),
                              reads=[Tpx[q]], writes=[TxT])
                for k in range(NK):
                    cx.op("pe", lambda: nc.tensor.matmul(out=plg[:, :], lhsT=xT[:, k, :], rhs=wr[:, k, :], start=(k == 0), stop=(k == NK - 1)),
                          reads=[TxT, Twr], writes=[Tplg], signal=(k == NK - 1))
                cx.op("dve", lambda: nc.vector.scalar_tensor_tensor(out=lgA[:, i, :], in0=plg[:, :], scalar=sm[:, 1:2], in1=bias[:, :], op0=ALU.mult, op1=ALU.add),
                      reads=[Tplg, Tsm, Tbias], writes=[TlgA])

            for i in range(NT):
                a_front(i)
                a_back(i)
            V = nc.vector
            bc = lambda ap2, n: ap2.unsqueeze(2).broadcast_to([128, NT, n])
            coarse = lgA[:, :, 0:8]
            cx.op("dve", lambda: V.tensor_reduce(out=r16[:, 0, :], in_=coarse, axis=AX.X, op=ALU.max), reads=[TlgA], writes=[Tr16])
            cx.op("dve", lambda: V.tensor_tensor(out=gm[:, :, :], in0=coarse, in1=bc(r16[:, 0, :], 8), op=ALU.is_equal), reads=[TlgA, Tr16], writes=[Tgm])
            cx.op("dve", lambda: V.tensor_tensor(out=ce[:, :, :], in0=coarse, in1=bc(r16[:, 0, :], 8), op=ALU.subtract), reads=[TlgA, Tr16], writes=[Tce])
            cx.op("act", lambda: nc.scalar.activation(out=ce[:, :, :], in_=ce[:, :, :], func=AF.Exp), reads=[Tce], writes=[Tce])
            cx.op("dve", lambda: V.tensor_reduce(out=r16[:, 1, :], in_=ce[:, :, :], axis=AX.X, op=ALU.add), reads=[Tce], writes=[Tr16])
            cx.op("dve", lambda: V.reciprocal(out=r16[:, 2, :], in_=r16[:, 1, :]), reads=[Tr16], writes=[Tr16])
            cx.op("dve", lambda: V.tensor_scalar(out=gm[:, :, :], in0=gm[:, :, :], scalar1=BIG, scalar2=-BIG, op0=ALU.mult, op1=ALU.add),
                  reads=[Tgm], writes=[Tgm])
            cx.op("dve", lambda: V.tensor_tensor(out=fm[:, :, :].rearrange("p t (a b) -> p t a b", b=8),
                                                 in0=lgA[:, :, 8:72].rearrange("p t (a b) -> p t a b", b=8),
                                                 in1=gm[:, :, :].unsqueeze(3).broadcast_to([128, NT, 8, 8]), op=ALU.add),
                  reads=[TlgA, Tgm], writes=[Tfm])
            cx.op("dve", lambda: V.tensor_reduce(out=r16[:, 3, :], in_=fm[:, :, :], axis=AX.X, op=ALU.max), reads=[Tfm], writes=[Tr16])
            cx.op("dve", lambda: V.tensor_tensor(out=m12[:, :, 0, :], in0=fm[:, :, :], in1=bc(r16[:, 3, :], NE), op=ALU.is_equal),
                  reads=[Tfm, Tr16], writes=[Tm12])
            cx.op("dve", lambda: V.scalar_tensor_tensor(out=fm2[:, :, :], in0=m12[:, :, 0, :], scalar=-BIG, in1=fm[:, :, :], op0=ALU.mult, op1=ALU.add),
                  reads=[Tm12, Tfm], writes=[Tfm2])
            cx.op("dve", lambda: V.tensor_reduce(out=r16[:, 4, :], in_=fm2[:, :, :], axis=AX.X, op=ALU.max), reads=[Tfm2], writes=[Tr16])
            cx.op("dve", lambda: V.tensor_tensor(out=m12[:, :, 1, :], in0=fm2[:, :, :], in1=bc(r16[:, 4, :], NE), op=ALU.is_equal),
                  reads=[Tfm2, Tr16], writes=[Tm12])
            cx.op("dve", lambda: V.tensor_tensor(out=maskall[:, :, :], in0=m12[:, :, 0, :], in1=m12[:, :, 1, :], op=ALU.add), reads=[Tm12], writes=[Tmask])
            cx.op("dve", lambda: V.tensor_tensor(out=r16[:, 5, :], in0=r16[:, 4, :], in1=r16[:, 3, :], op=ALU.subtract), reads=[Tr16], writes=[Tr16])
            cx.op("act", lambda: nc.scalar.activation(out=r16[:, 5, :], in_=r16[:, 5, :], func=AF.Exp), reads=[Tr16], writes=[Tr16])
            cx.op("dve", lambda: V.tensor_scalar(out=r16[:, 6, :], in0=r16[:, 5, :], scalar1=1.0, scalar2=None, op0=ALU.add), reads=[Tr16], writes=[Tr16])
            cx.op("dve", lambda: V.reciprocal(out=r16[:, 6, :], in_=r16[:, 6, :]), reads=[Tr16], writes=[Tr16])
            cx.op("dve", lambda: V.tensor_tensor(out=cw[:, :, 0], in0=r16[:, 6, :], in1=r16[:, 2, :], op=ALU.mult), reads=[Tr16], writes=[Tcw])
            cx.op("dve", lambda: V.tensor_tensor(out=cw[:, :, 1], in0=cw[:, :, 0], in1=r16[:, 5, :], op=ALU.mult), reads=[Tr16, Tcw], writes=[Tcw])
            cx.barrier()

        with ExitStack() as s2:
            sb2 = lambda n, s, d: s2.enter_context(nc.sbuf_tensor(U(n), s, d))
            tmp = sb2("mb_tmp", [128, NT, NE], F32); Ttmp = T("mb_tmp")
            tmp2 = sb2("mb_tmp2", [128, NT, NE], F32); Ttmp2 = T("mb_tmp2")
            tmp3 = sb2("mb_tmp3", [128, NT, NE], F32); Ttmp3 = T("mb_tmp3")
            iota1 = sb2("mb_iota1", [128, NE], F32); Tiota1 = T("mb_iota1")
            s2f = sb2("mb_s2f", [128, NT, 2], F32); Ts2f = T("mb_s2f")
            s2i = sb2("mb_s2i", [128, NT, 2], I32); Ts2i = T("mb_s2i")
            pcs = [s2.enter_context(nc.psum_tensor(U(f"mb_pc{i}"), [128, NE], F32)) for i in range(2)]; Tpc = [T(f"mb_pc{i}") for i in range(2)]
            V = nc.vector
            tokbuf, Ttokbuf = g.tokbuf, g.Ttokbuf
            cx.dma("sp", tokbuf[0:CAP * NE, :].rearrange("(c e) o -> c (e o)", e=NE), g.fill2048[:, :], reads=[g.Tconst], writes=[Ttokbuf], dst=Ttokbuf)
            for i in range(NT):
                pc = pcs[i % 2]; Tp = Tpc[i % 2]
                for j in range(i):
                    cx.op("pe", lambda: nc.tensor.matmul(out=pc[:, :], lhsT=onesb[:, :], rhs=maskall[:, j, :], start=(j == 0), stop=False),
                          reads=[Tones, Tmask], writes=[Tp], signal=False)
                cx.op("pe", lambda: nc.tensor.matmul(out=pc[:, :], lhsT=g.tri[:, :], rhs=maskall[:, i, :], start=(i == 0), stop=True),
                      reads=[g.Tconst, Tmask], writes=[Tp])
                cx.op("act", lambda: nc.scalar.copy(out=cum[:, i, :], in_=pc[:, :]), reads=[Tp], writes=[Tcum])
            cx.op("dve", lambda: V.tensor_scalar(out=iota1[:, :], in0=g.iota_e[:, :], scalar1=1.0 / 128.0, scalar2=None, op0=ALU.mult), reads=[g.Tconst], writes=[Tiota1])
            cx.op("dve", lambda: V.tensor_tensor(out=tmp[:, :, :], in0=cum[:, :, :], in1=maskall[:, :, :], op=ALU.subtract), reads=[Tcum, Tmask], writes=[Ttmp])
            cx.op("dve", lambda: V.scalar_tensor_tensor(out=tmp3[:, :, :], in0=tmp[:, :, :], scalar=64.0, in1=iota1[:, :].unsqueeze(1).broadcast_to([128, NT, NE]),
                                                        op0=ALU.mult, op1=ALU.add), reads=[Ttmp, Tiota1], writes=[Ttmp3])
            cx.op("dve", lambda: V.tensor_tensor(out=tmp[:, :, :], in0=tmp[:, :, :], in1=g.iota_e[:, :].unsqueeze(1).broadcast_to([128, NT, NE]), op=ALU.add),
                  reads=[Ttmp, g.Tconst], writes=[Ttmp])
            for kk in range(2):
                cx.op("dve", lambda: V.tensor_tensor(out=tmp2[:, :, :], in0=tmp[:, :, :], in1=m12[:, :, kk, :], op=ALU.mult), reads=[Ttmp, Tm12], writes=[Ttmp2])
                cx.op("dve", lambda: V.tensor_reduce(out=sidf[:, :, kk], in_=tmp2[:, :, :], axis=AX.X, op=ALU.add), reads=[Ttmp2], writes=[Tsidf])
                cx.op("dve", lambda: V.tensor_tensor(out=tmp2[:, :, :], in0=tmp3[:, :, :], in1=m12[:, :, kk, :], op=ALU.mult), reads=[Ttmp3, Tm12], writes=[Ttmp2])
                cx.op("dve", lambda: V.tensor_reduce(out=s2f[:, :, kk], in_=tmp2[:, :, :], axis=AX.X, op=ALU.add), reads=[Ttmp2], writes=[Ts2f])
            cx.op("dve", lambda: V.tensor_copy(out=sidx[:, :, :], in_=sidf[:, :, :]), reads=[Tsidf], writes=[Tsidx])
            cx.op("dve", lambda: V.tensor_copy(out=s2i[:, :, :], in_=s2f[:, :, :]), reads=[Ts2f], writes=[Ts2i])
            pre = dict(Ttokbuf.w)
            for k_, v_ in pre.items():
                cx._wait("pool", k_, v_)
            cx._deps("pool", [Ts2i, g.Tconst], [])
            for i in range(NT):
                for kk in range(2):
                    inst = nc.gpsimd.indirect_dma_start(out=tokbuf[:, :], out_offset=bass.IndirectOffsetOnAxis(ap=s2i[:, i, kk:kk + 1], axis=0),
                                                        in_=g.tokid[:, i:i + 1], in_offset=None)
                    Ttokbuf.dcount += 16
                    inst.then_inc(Ttokbuf.dsem, 16)
                    cx.log["pool"].append(("s", ("d", Ttokbuf.name), 16))
            kd = ("d", Ttokbuf.name)
            Ttokbuf.w[kd] = Ttokbuf.dcount
            Ts2i.r[kd] = Ttokbuf.dcount
            cx.dma("sp", tok[:, :], tokbuf[0:CAP * NE, :].rearrange("(c e) o -> c (e o)", e=NE), reads=[Ttokbuf], writes=[Ttok], dst=Ttok)
            if getattr(g, "dbg", None) is not None and layer == 0:
                Td = T("dbgout")
                cx.dma("sp", g.dbg["tok"][:, :], tok[:, :], reads=[Ttok], writes=[Td], dst=Td)
            cx.barrier()

        sAB.close()
        with ExitStack() as s2:
            sb2 = lambda n, s, d: s2.enter_context(nc.sbuf_tensor(U(n), s, d))
            wg = sb2("mc_wg", [128, NK, DE], BF16); Twg = T("mc_wg")
            wu = sb2("mc_wu", [128, NK, DE], BF16); Twu = T("mc_wu")
            wd = sb2("mc_wd", [128, 4, D], BF16); Twd = T("mc_wd")
            xe = [sb2(f"mc_xe{i}", [128, D], BF16) for i in range(2)]; Txe = [T(f"mc_xe{i}") for i in range(2)]
            xeT = sb2("mc_xeT", [128, NK, 128], BF16); TxeT = T("mc_xeT")
            asb = sb2("mc_a", [128, DE], F32); Ta = T("mc_a")
            abf = sb2("mc_abf", [128, DE], BF16); Tabf = T("mc_abf")
            aT = sb2("mc_aT", [128, 4, 128], BF16); TaT = T("mc_aT")
            ysb = [sb2(f"mc_y{i}", [128, D], BF16) for i in range(2)]; Tysb = [T(f"mc_y{i}") for i in range(2)]
            ptr = [s2.enter_context(nc.psum_tensor(U(f"mc_ptr{i}"), [128, 1024], BF16)) for i in range(2)]; Tptr = [T(f"mc_ptr{i}") for i in range(2)]
            pg = s2.enter_context(nc.psum_tensor(U("mc_pg"), [128, 512], F32)); Tpg = T("mc_pg")
            pu = s2.enter_context(nc.psum_tensor(U("mc_pu"), [128, 512], F32)); Tpu = T("mc_pu")
            py = [s2.enter_context(nc.psum_tensor(U(f"mc_py{i}"), [128, 512], F32)) for i in range(4)]; Tpy = [T(f"mc_py{i}") for i in range(4)]
            wdst = [(wg[:, :, :].rearrange("p k n -> p (k n)"), Twg), (wu[:, :, :].rearrange("p k n -> p (k n)"), Twu),
                    (wd[:, :, :].rearrange("p k n -> p (k n)"), Twd)]

            def cast_w(e, j):
                q = (3 * e + j) % NSLOT
                dstap, Td = wdst[j]
                cx.op("act", lambda: nc.scalar.copy(out=dstap[:, 0:4096], in_=stg[q][:, 0:4096]), reads=[Tstg[q]], writes=[Td])
                cx.op("dve", lambda: nc.vector.tensor_copy(out=dstap[:, 4096:8192], in_=stg[q][:, 4096:8192]), reads=[Tstg[q]], writes=[Td])

            def gather_x(e):
                cx.gather(xe[e % 2][:, :], hs[:, :], tok[:, e:e + 1], reads=[Ths, Ttok], writes=[Txe[e % 2]], dst=Txe[e % 2])

            gather_x(0)
            npieces = 3 * NE

            def load_ahead(e, j):
                q = 3 * e + j + NSLOT
                if q < npieces:
                    load_w(q // 3, q % 3)

            for e in range(NE):
                b = e % 2
                if e + 1 < NE:
                    gather_x(e + 1)
                cast_w(e, 0)
                load_ahead(e, 0)
                xv = xe[b][:, :].rearrange("s (p k) -> s k p", k=NK)
                for half in range(2):
                    for c in range(8):
                        k = half * 8 + c
                        cx.op("pe", lambda: nc.tensor.transpose(out=ptr[half][:, c * 128:(c + 1) * 128], in_=xv[:, k, :], identity=g.identb[:, :]),
                              reads=[Txe[b], g.Tconst], writes=[Tptr[half]], signal=(c == 7))
                    if half == 0:
                        cx.op("act", lambda: nc.scalar.copy(out=xeT[:, 0:8, :], in_=ptr[0][:, :].rearrange("p (c t) -> p c t", c=8)), reads=[Tptr[0]], writes=[TxeT])
                    else:
                        cx.op("dve", lambda: nc.vector.tensor_copy(out=xeT[:, 8:16, :], in_=ptr[1][:, :].rearrange("p (c t) -> p c t", c=8)), reads=[Tptr[1]], writes=[TxeT])
                for k in range(NK):
                    cx.op("pe", lambda: nc.tensor.matmul(out=pg[:, :], lhsT=xeT[:, k, :], rhs=wg[:, k, :], start=(k == 0), stop=(k == NK - 1)),
                          reads=[TxeT, Twg], writes=[Tpg], signal=(k == NK - 1))
                cast_w(e, 1)
                load_ahead(e, 1)
                for k in range(NK):
                    cx.op("pe", lambda: nc.tensor.matmul(out=pu[:, :], lhsT=xeT[:, k, :], rhs=wu[:, k, :], start=(k == 0), stop=(k == NK - 1)),
                          reads=[TxeT, Twu], writes=[Tpu], signal=(k == NK - 1))
                cx.op("act", lambda: nc.scalar.activation(out=asb[:, :], in_=pg[:, :], func=AF.Silu), reads=[Tpg], writes=[Ta])
                cx.op("dve", lambda: nc.vector.tensor_tensor(out=abf[:, :], in0=pu[:, :], in1=asb[:, :], op=ALU.mult), reads=[Tpu, Ta], writes=[Tabf])
                av = abf[:, :].rearrange("s (p k) -> s k p", k=4)
                for c in range(4):
                    cx.op("pe", lambda: nc.tensor.transpose(out=ptr[0][:, c * 128:(c + 1) * 128], in_=av[:, c, :], identity=g.identb[:, :]),
                          reads=[Tabf, g.Tconst], writes=[Tptr[0]], signal=(c == 3))
                cx.op("act", lambda: nc.scalar.copy(out=aT[:, :, :], in_=ptr[0][:, 0:512].rearrange("p (c t) -> p c t", c=4)), reads=[Tptr[0]], writes=[TaT])
                cast_w(e, 2)
                load_ahead(e, 2)
                yb = ysb[b]; Tyb = Tysb[b]
                for nb in range(4):
                    for f in range(4):
                        cx.op("pe", lambda: nc.tensor.matmul(out=py[nb][:, :], lhsT=aT[:, f, :], rhs=wd[:, f, nb * 512:(nb + 1) * 512], start=(f == 0), stop=(f == 3)),
                              reads=[TaT, Twd], writes=[Tpy[nb]], signal=(f == 3))
                    if nb % 2 == 0:
                        cx.op("act", lambda: nc.scalar.copy(out=yb[:, nb * 512:(nb + 1) * 512], in_=py[nb][:, :]), reads=[Tpy[nb]], writes=[Tyb])
                    else:
                        cx.op("dve", lambda: nc.vector.tensor_copy(out=yb[:, nb * 512:(nb + 1) * 512], in_=py[nb][:, :]), reads=[Tpy[nb]], writes=[Tyb])
                cx.dma("sp", ys[e * CAP:(e + 1) * CAP, :], yb[:, :], reads=[Tyb], writes=[Tys], dst=Tys)
            cx.barrier()

        with ExitStack() as s2:
            sb2 = lambda n, s, d: s2.enter_context(nc.sbuf_tensor(U(n), s, d))
            xts = [sb2(f"md_xt{i}", [128, D], F32) for i in range(3)]; Txt = [T(f"md_xt{i}") for i in range(3)]
            ya = [sb2(f"md_ya{i}", [128, D], BF16) for i in range(2)]; Tya = [T(f"md_ya{i}") for i in range(2)]
            yb_ = [sb2(f"md_yb{i}", [128, D], BF16) for i in range(2)]; Tyb_ = [T(f"md_yb{i}") for i in range(2)]
            cx.dma("sp", xts[0][:, :], x_src[0:128, :], reads=[Tsrc], writes=[Txt[0]], dst=Txt[0])
            for i in range(NT):
                b = i % 2
                c3 = i % 3
                if i + 1 < NT:
                    n3 = (i + 1) % 3
                    cx.dma("sp", xts[n3][:, :], x_src[(i + 1) * 128:(i + 2) * 128, :], reads=[Tsrc], writes=[Txt[n3]], dst=Txt[n3])
                cx.gather(ya[b][:, :], ys[:, :], sidx[:, i, 0:1], reads=[Tys, Tsidx], writes=[Tya[b]], dst=Tya[b])
                cx.gather(yb_[b][:, :], ys[:, :], sidx[:, i, 1:2], reads=[Tys, Tsidx], writes=[Tyb_[b]], dst=Tyb_[b])
                cx.op("dve", lambda: nc.vector.scalar_tensor_tensor(out=xts[c3][:, :], in0=ya[b][:, :], scalar=cw[:, i, 0:1], in1=xts[c3][:, :], op0=ALU.mult, op1=ALU.add),
                      reads=[Tya[b], Tcw, Txt[c3]], writes=[Txt[c3]])
                cx.op("dve", lambda: nc.vector.scalar_tensor_tensor(out=xts[c3][:, :], in0=yb_[b][:, :], scalar=cw[:, i, 1:2], in1=xts[c3][:, :], op0=ALU.mult, op1=ALU.add),
                      reads=[Tyb_[b], Tcw, Txt[c3]], writes=[Txt[c3]])
                cx.dma("sp", x_dst[i * 128:(i + 1) * 128, :], xts[c3][:, :], reads=[Txt[c3]], writes=[Tdst], dst=Tdst)
            cx.barrier()


def build_program(phases=(1, 2, 3, 4, 5, 6), dbg=False):
    nc = bass.Bass("TRN2", target_bir_lowering=False)
    g = G()
    g.nc = nc
    dram = {}
    for name, shape in IN_SPECS.items():
        dram[name] = nc.dram_tensor(name, shape, F32, kind="ExternalInput").ap()
    for name, (shape, dt) in CONST_SPECS.items():
        dram[name] = nc.dram_tensor(name, shape, dt, kind="ExternalInput").ap()
    g.dram = dram
    kind = "ExternalOutput" if dbg else "Internal"
    y = nc.dram_tensor("y", [S, D], F32, kind="ExternalOutput").ap()
    x1 = nc.dram_tensor("x1", [S, D], F32, kind=kind).ap()
    x2 = nc.dram_tensor("x2", [S, D], F32, kind=kind).ap()
    x3 = nc.dram_tensor("x3", [S, D], F32, kind=kind).ap()
    mixT = nc.dram_tensor("mixT", [AW, S], BF16, kind=kind).ap()
    fT = nc.dram_tensor("fT", [D, S], BF16, kind=kind).ap()
    if dbg:
        g.dbg = {"tok": nc.dram_tensor("dbg_tok", [128, NE], I32, kind="ExternalOutput").ap(),
                 "sidx": nc.dram_tensor("dbg_sidx", [128, NT * 2], I32, kind="ExternalOutput").ap(),
                 "cum": nc.dram_tensor("dbg_cum", [128, NT * NE], F32, kind="ExternalOutput").ap(),
                 "cw": nc.dram_tensor("dbg_cw", [128, NT * 2], F32, kind="ExternalOutput").ap(),
                 "m12": nc.dram_tensor("dbg_m12", [128, NT * 2 * NE], F32, kind="ExternalOutput").ap()}
    g.hs = nc.dram_tensor("hs", [S + 1, D], BF16, kind="Internal").ap()
    g.ys = nc.dram_tensor("ys", [NE * CAP + S, D], BF16, kind="Internal").ap()
    g.tokbuf = nc.dram_tensor("tokbuf", [S * NE, 1], I32, kind="Internal").ap()
    g.Ttokbuf = T("tokbuf")
    g.Ths, g.Tys = T("hs"), T("ys")
    Tx0, Tx1, Tx2, Tx3, Ty, Tmix, TfT = T("x0"), T("x1"), T("x2"), T("x3"), T("y"), T("mixT"), T("fT")
    with ExitStack() as st:
        cx = Ctx(nc, st)
        g.cx = cx
        G.last_cx = cx
        sb = lambda n, s, d: st.enter_context(nc.sbuf_tensor(U(n), s, d))
        g.Tconst = T("consts")
        for name in ("identb", "identf", "rotP", "bmask", "tri", "iota_e", "tokid", "fill2048"):
            shape, dt = CONST_SPECS[name]
            t = sb("c_" + name, shape, dt)
            setattr(g, name, t)
            cx.dma("sp", t[:, :], dram[name][:, :], writes=[g.Tconst], dst=g.Tconst)
        xin = dram["x"]
        if 1 in phases:
            phase_attn(g, xin, mixT, Tmix)
        if 2 in phases:
            phase_outproj(g, mixT, Tmix, NH, dram["w_attn_out"], xin, Tx0, x1, Tx1)
        if 3 in phases:
            phase_moe(g, 0, x1, Tx1, x2, Tx2)
        if 4 in phases:
            phase_fourier(g, x2, fT, TfT)
        if 5 in phases:
            phase_outproj(g, fT, TfT, NK, dram["w_fourier_out"], x2, Tx2, x3, Tx3)
        if 6 in phases:
            phase_moe(g, 1, x3, Tx3, y, Ty)
        cx.barrier()
        cx.final_wait([t for t in (Ty, Tx1, Tx2, Tx3, Tmix, TfT) if t.dsem is not None])
    return nc


_CONSTS = None


def make_in_map(inputs, b, consts):
    m = {}
    for name, shape in IN_SPECS.items():
        a = np.asarray(inputs[name], dtype=np.float32)
        if name == "x":
            a = a[b]
        m[name] = np.ascontiguousarray(a.reshape(shape))
    m.update(consts)
    return m


def kernel(**inputs):
    global _CONSTS
    if _CONSTS is None:
        _CONSTS = make_consts()
    n = 8
    nc = build_program()
    in_maps = [make_in_map(inputs, b, _CONSTS) for b in range(n)]
    res = run_bass_kernel_spmd(nc, in_maps, core_ids=list(range(n)))
    out = np.stack([np.asarray(res.results[b]["y"], dtype=np.float32) for b in range(n)], axis=0)
    return out
```
